# Optimizing a Trainium2 kernel written in Bass

```python
import math
import jax, jax.numpy as jnp
from jax import lax
import numpy as np


D_MODEL = 1024
BATCH = 16
SEQ = 2048
DEPTH = 4

D_ATTN = 512
ATTN_HEADS = 4
ATTN_QK_DIM = 64
ATTN_V_DIM = 128
Q_BLOCK = 128
REL_BUCKETS = 32
REL_MAX_DISTANCE = 128
SUBLN_EPS = 1e-5
D_HYENA = 512
HYENA_ORDER = 2
HYENA_SHORT_CONV = 3
N_DIRS = 2
FILTER_BANDS = 16
FILTER_EMB = 2 * FILTER_BANDS + 1
FILTER_WIDTH = 64
FAST_DECAY_PCT = 0.3
SLOW_DECAY_PCT = 1.5
DECAY_TARGET = 1e-2
NORM_EPS = 1e-6
FILTER_CH = N_DIRS * HYENA_ORDER * D_HYENA
W_IN_COLS = 4 * D_ATTN + 4 * D_HYENA + 2 * D_MODEL

kernel_name = "hybrid_diffattn_hyena_encoder"


def rms_norm(x, gain, eps):
    x32 = x.astype(jnp.float32)
    y = x32 * lax.rsqrt(jnp.mean(x32 * x32, axis=-1, keepdims=True) + eps)
    return (y * gain.astype(jnp.float32)).astype(x.dtype)


def t5_bucket(rel):
    nb = REL_BUCKETS // 2
    max_exact = nb // 2
    ret = jnp.where(rel > 0, nb, 0)
    n = jnp.abs(rel)
    nf = jnp.maximum(n, 1).astype(jnp.float32)
    large = max_exact + (jnp.log(nf / max_exact) / math.log(REL_MAX_DISTANCE / max_exact)
                         * (nb - max_exact)).astype(jnp.int32)
    large = jnp.minimum(large, nb - 1)
    return ret + jnp.where(n < max_exact, n, large)


def diff_attention(q, k, v, lam, rel_bias):
    B, S = q.shape[0], q.shape[1]
    nblk = S // Q_BLOCK
    qb = (q * (ATTN_QK_DIM ** -0.5)).reshape(B, nblk, Q_BLOCK, ATTN_HEADS, 2, ATTN_QK_DIM)
    qb = qb.transpose(1, 0, 3, 4, 2, 5)
    kt = k.transpose(0, 2, 3, 1, 4)
    vt = v.transpose(0, 2, 1, 3)
    k_pos = jnp.arange(S, dtype=jnp.int32)

    def block(args):
        q_blk, i = args
        q_pos = i * Q_BLOCK + jnp.arange(Q_BLOCK, dtype=jnp.int32)
        bucket = t5_bucket(k_pos[None, :] - q_pos[:, None])
        bias = rel_bias[bucket].astype(jnp.float32).transpose(2, 0, 1)
        logits = jnp.einsum('bhcqd,bhckd->bhcqk', q_blk, kt).astype(jnp.float32)
        p = jax.nn.softmax(logits + bias[None, :, None], axis=-1)
        w = p[:, :, 0] - lam * p[:, :, 1]
        return jnp.einsum('bhqk,bhkd->bhqd', w.astype(vt.dtype), vt)

    out = lax.map(block, (qb, jnp.arange(nblk, dtype=jnp.int32)))
    return out.transpose(1, 0, 3, 2, 4).reshape(B, S, ATTN_HEADS, ATTN_V_DIM)


def short_conv3(u, w, b):
    up = jnp.pad(u, ((0, 0), (1, 1), (0, 0)))
    return up[:, :-2] * w[0] + up[:, 1:-1] * w[1] + up[:, 2:] * w[2] + b


def hyena_filters(L, w1, b1, w2, b2, w3, b3, freq, w_out, deltas):
    t = jnp.linspace(0.0, 1.0, L, dtype=jnp.float32)[:, None]
    t_res = jnp.arange(L, dtype=jnp.float32)[:, None]
    f = jnp.linspace(1e-4, FILTER_BANDS - 1, FILTER_BANDS, dtype=jnp.float32)[None, :]
    ang = 2.0 * math.pi * t_res * f / L
    z = jnp.concatenate([t, jnp.cos(ang), -jnp.sin(ang)], axis=-1)
    a = jnp.sin(freq * (z @ w1 + b1))
    a = jnp.sin(freq * (a @ w2 + b2))
    a = jnp.sin(freq * (a @ w3 + b3))
    decay = jnp.exp(-t * jnp.abs(deltas)[None, :])
    h = ((a @ w_out) * decay).astype(jnp.float32).reshape(L, N_DIRS, HYENA_ORDER, D_HYENA)
    fwd, bwd = h[:, 0], h[:, 1]
    two_sided = jnp.concatenate(
        [fwd, jnp.zeros((1, HYENA_ORDER, D_HYENA), jnp.float32), bwd[:0:-1]], axis=0)
    return jnp.fft.rfft(two_sided, axis=0)


def fft_long_conv(u, h_f, bias):
    L = u.shape[1]
    u32 = u.astype(jnp.float32)
    y = jnp.fft.irfft(jnp.fft.rfft(u32, n=2 * L, axis=1) * h_f[None], n=2 * L, axis=1)[:, :L]
    return (y + u32 * bias.astype(jnp.float32)).astype(u.dtype)


def setup_inputs(seed: int = 0) -> dict:
    key = jax.random.key(seed)
    ks = jax.random.split(key, 26)

    def nrm(k, shape, s):
        return jax.random.normal(k, shape, jnp.float32) * s

    deltas0 = jnp.linspace(math.log(FAST_DECAY_PCT) / DECAY_TARGET,
                           math.log(SLOW_DECAY_PCT) / DECAY_TARGET, FILTER_CH, dtype=jnp.float32)
    return {
        "x": nrm(ks[0], (BATCH, SEQ, D_MODEL), 1.0),
        "rel_bias": nrm(ks[1], (REL_BUCKETS, ATTN_HEADS), 0.1),
        "pre_norm": 1.0 + nrm(ks[2], (DEPTH, D_MODEL), 0.02),
        "post_norm": 1.0 + nrm(ks[3], (DEPTH, D_MODEL), 0.02),
        "w_in": nrm(ks[4], (DEPTH, D_MODEL, W_IN_COLS), D_MODEL ** -0.5),
        "lam_q1": nrm(ks[5], (DEPTH, ATTN_QK_DIM), 0.1),
        "lam_k1": nrm(ks[6], (DEPTH, ATTN_QK_DIM), 0.1),
        "lam_q2": nrm(ks[7], (DEPTH, ATTN_QK_DIM), 0.1),
        "lam_k2": nrm(ks[8], (DEPTH, ATTN_QK_DIM), 0.1),
        "subln": 1.0 + nrm(ks[9], (DEPTH, ATTN_V_DIM), 0.02),
        "conv_w": nrm(ks[10], (DEPTH, HYENA_SHORT_CONV, 3 * D_HYENA), HYENA_SHORT_CONV ** -0.5),
        "conv_b": nrm(ks[11], (DEPTH, 3 * D_HYENA), 0.02),
        "flt_w1": nrm(ks[12], (DEPTH, FILTER_EMB, FILTER_WIDTH), FILTER_EMB ** -0.5),
        "flt_b1": nrm(ks[13], (DEPTH, FILTER_WIDTH), 0.1),
        "flt_w2": nrm(ks[14], (DEPTH, FILTER_WIDTH, FILTER_WIDTH), FILTER_WIDTH ** -0.5),
        "flt_b2": nrm(ks[15], (DEPTH, FILTER_WIDTH), 0.1),
        "flt_w3": nrm(ks[16], (DEPTH, FILTER_WIDTH, FILTER_WIDTH), FILTER_WIDTH ** -0.5),
        "flt_b3": nrm(ks[17], (DEPTH, FILTER_WIDTH), 0.1),
        "flt_freq": 1.0 + nrm(ks[18], (DEPTH, FILTER_WIDTH), 0.01),
        "flt_w_out": nrm(ks[19], (DEPTH, FILTER_WIDTH, FILTER_CH), 0.1 * FILTER_WIDTH ** -0.5),
        "flt_deltas": deltas0[None, :] + nrm(ks[20], (DEPTH, FILTER_CH), 0.01),
        "flt_bias": nrm(ks[21], (DEPTH, HYENA_ORDER, D_HYENA), 0.1),
        "w_pa": nrm(ks[22], (DEPTH, D_ATTN, D_MODEL), D_ATTN ** -0.5),
        "w_ph": nrm(ks[23], (DEPTH, D_HYENA, D_MODEL), D_HYENA ** -0.5),
        "w_out": nrm(ks[24], (DEPTH, D_MODEL, D_MODEL), D_MODEL ** -0.5),
    }


def reference(x, rel_bias, pre_norm, post_norm, w_in, lam_q1, lam_k1, lam_q2, lam_k2, subln,
              conv_w, conv_b, flt_w1, flt_b1, flt_w2, flt_b2, flt_w3, flt_b3, flt_freq,
              flt_w_out, flt_deltas, flt_bias, w_pa, w_ph, w_out):
    B, S, _ = x.shape
    widths = (D_ATTN, D_ATTN, D_ATTN, D_ATTN, 3 * D_HYENA, D_HYENA, D_MODEL, D_MODEL)
    splits = [int(c) for c in np.cumsum(widths)[:-1]]
    for l in range(DEPTH):
        lam_init = 0.8 - 0.6 * math.exp(-0.3 * l)
        h = rms_norm(x, pre_norm[l], NORM_EPS)
        proj = h @ w_in[l]
        q, k, v, z_a, u_h, z_h, g_a, g_h = jnp.split(proj, splits, axis=-1)

        lam = (jnp.exp(jnp.sum(lam_q1[l].astype(jnp.float32) * lam_k1[l].astype(jnp.float32)))
               - jnp.exp(jnp.sum(lam_q2[l].astype(jnp.float32) * lam_k2[l].astype(jnp.float32)))
               + lam_init)
        o = diff_attention(q.reshape(B, S, ATTN_HEADS, 2, ATTN_QK_DIM),
                           k.reshape(B, S, ATTN_HEADS, 2, ATTN_QK_DIM),
                           v.reshape(B, S, ATTN_HEADS, ATTN_V_DIM), lam, rel_bias)
        o = rms_norm(o, subln[l], SUBLN_EPS) * (1.0 - lam_init)
        y_a = o.reshape(B, S, D_ATTN) * jax.nn.silu(z_a)

        u_h = short_conv3(u_h, conv_w[l], conv_b[l])
        hv, hx1, hx2 = jnp.split(u_h, 3, axis=-1)
        filt = hyena_filters(S, flt_w1[l], flt_b1[l], flt_w2[l], flt_b2[l], flt_w3[l], flt_b3[l],
                             flt_freq[l], flt_w_out[l], flt_deltas[l])
        z = fft_long_conv(hv, filt[:, 0], flt_bias[l, 0]) * hx1
        z = fft_long_conv(z, filt[:, 1], flt_bias[l, 1]) * hx2
        y_h = z * jax.nn.silu(z_h)

        m = jax.nn.sigmoid(g_a) * (y_a @ w_pa[l]) + jax.nn.sigmoid(g_h) * (y_h @ w_ph[l])
        x = x + rms_norm(m @ w_out[l], post_norm[l], NORM_EPS)
    return x
```

```python
import math
import contextlib
import numpy as np
import ml_dtypes
import concourse.bass as bass
import concourse.mybir as mybir
from concourse.bass_utils import run_bass_kernel_spmd

F32 = mybir.dt.float32
BF16 = mybir.dt.bfloat16
I32 = mybir.dt.int32
AF = mybir.ActivationFunctionType
ALU = mybir.AluOpType
AX = mybir.AxisListType

P = 128
S = 2048
D = 1024
KT = 8
NBC = 2
NCORES = 8
DEPTH = 4
WCOLS = 6144
CW = 256
NH = 512 // CW
NFFT = 4096
R_B = 1280
TBW = 1152
NORM_EPS = 1e-6
SUBLN_EPS = 1e-5
TWO_PI = 2.0 * math.pi
NCOLP = 64
NROWP = 4352


class Sem:
    def __init__(self, h):
        self.h = h
        self.v = 0
        self.pending = []
        self.closed_at = 0


class Buf:
    __slots__ = ("w", "r")

    def __init__(self):
        self.w = {}
        self.r = {}


class Sched:
    def __init__(self, nc, es):
        self.nc = nc
        self.es = es
        self.E = {"pe": nc.tensor, "act": nc.scalar, "dve": nc.vector, "pool": nc.gpsimd, "sp": nc.sync}
        self.sem = {k: Sem(es.enter_context(nc.semaphore("e_" + k))) for k in self.E}
        self.seen = {k: {} for k in self.E}
        self.dsems = []
        self.ninst = 0

    def newsem(self, name):
        x = Sem(self.es.enter_context(self.nc.semaphore(name)))
        self.dsems.append(x)
        return x

    def _wait(self, eng, tok):
        sem, val, owner = tok
        if owner == eng and eng == "pe":
            return
        if self.seen[eng].get(id(sem), 0) >= val:
            return
        self.E[eng].wait_ge(sem.h, val)
        self.seen[eng][id(sem)] = val

    def _deps(self, eng, reads, writes):
        for b in reads:
            for t in b.w.values():
                self._wait(eng, t)
        for b in writes:
            for t in b.w.values():
                self._wait(eng, t)
            for t in b.r.values():
                self._wait(eng, t)

    def _done(self, tok, key, reads, writes):
        for b in reads:
            b.r[key] = tok
        for b in writes:
            b.w[key] = tok
            b.r = {}

    def op(self, eng, fn, reads=(), writes=()):
        self._deps(eng, reads, writes)
        inst = fn(self.E[eng])
        sem = self.sem[eng]
        sem.v += 1
        inst.then_inc(sem.h, 1)
        self.ninst += 1
        self._done((sem, sem.v, eng), id(sem), reads, writes)

    def group(self, eng, fns, reads=(), writes=()):
        self._deps(eng, reads, writes)
        for f in fns[:-1]:
            f(self.E[eng])
        inst = fns[-1](self.E[eng])
        sem = self.sem[eng]
        sem.v += 1
        inst.then_inc(sem.h, 1)
        self.ninst += len(fns)
        self._done((sem, sem.v, eng), id(sem), reads, writes)

    def dma(self, q, out, in_, sem, reads=(), writes=(), shared=False):
        self._deps(q, reads, writes)
        if shared:
            if sem.closed_at > 0:
                self._wait(q, (sem, sem.closed_at, None))
            sem.pending.extend(writes)
        self.E[q].dma_start(out=out, in_=in_).then_inc(sem.h, 16)
        sem.v += 16
        self.ninst += 1
        self._done((sem, sem.v, None), id(sem), reads, writes)

    def flush(self, sem):
        tok = (sem, sem.v, None)
        for b in sem.pending:
            b.w[id(sem)] = tok
        sem.pending = []
        sem.closed_at = sem.v

    def barrier(self):
        toks = [(sm, sm.v, None) for sm in list(self.sem.values()) + self.dsems if sm.v > 0]
        for e in self.E:
            for t in toks:
                self._wait(e, t)


def _t5_bucket_np(rel):
    nb = 16
    max_exact = 8
    ret = np.where(rel > 0, nb, 0)
    n = np.abs(rel)
    nf = np.maximum(n, 1).astype(np.float32)
    large = max_exact + (np.log(nf / np.float32(max_exact)) / np.float32(math.log(128 / max_exact))
                         * np.float32(nb - max_exact)).astype(np.int32)
    large = np.minimum(large, nb - 1)
    return ret + np.where(n < max_exact, n, large)


_CONST_CACHE = {}


def _consts():
    if _CONST_CACHE:
        return _CONST_CACHE
    s = np.arange(S, dtype=np.float64)
    f = np.arange(2048, dtype=np.float64) + 0.5
    theta = 2.0 * np.pi * np.outer(s, f) / NFFT
    fwm = np.concatenate([np.cos(theta), -np.sin(theta)], axis=1)
    fw = fwm.reshape(16, 128, 32, 128).transpose(2, 1, 0, 3)
    ivm = (2.0 / NFFT) * fwm.T
    iv = ivm.reshape(2, 16, 128, 16, 128).transpose(3, 0, 2, 1, 4).reshape(32, 128, 16, 128)
    _CONST_CACHE["fw"] = np.ascontiguousarray(fw).astype(ml_dtypes.bfloat16)
    _CONST_CACHE["iv"] = np.ascontiguousarray(iv).astype(ml_dtypes.bfloat16)
    L = S
    t = np.linspace(0.0, 1.0, L, dtype=np.float32)
    t_res = np.arange(L, dtype=np.float32)[:, None]
    fb = np.linspace(1e-4, 15, 16, dtype=np.float32)[None, :]
    ang = (np.float32(2.0 * math.pi) * t_res * fb / np.float32(L)).astype(np.float32)
    z = np.concatenate([t[:, None], np.cos(ang.astype(np.float64)), -np.sin(ang.astype(np.float64))], axis=-1)
    _CONST_CACHE["zT"] = np.ascontiguousarray(z.T).astype(np.float32)
    ng = np.zeros((128, 17), np.float32)
    ng[:, 0:16] = (-t).reshape(16, 128).T
    ng[:, 16] = 1.0 - 2.0 * (np.arange(128) % 2)
    _CONST_CACHE["negt"] = ng
    rel = 639 - np.arange(R_B)
    bk = _t5_bucket_np(rel)
    oh = np.zeros((32, R_B), np.float32)
    oh[bk, np.arange(R_B)] = 1.0
    _CONST_CACHE["onehot"] = oh
    _CONST_CACHE["ident"] = np.eye(128).astype(ml_dtypes.bfloat16)
    return _CONST_CACHE


def _pack_params(inp):
    L = DEPTH
    colp = np.zeros((L, 128, NCOLP), np.float32)
    rowp = np.zeros((L, NROWP), np.float32)
    for l in range(L):
        colp[l, :, 0:8] = inp["pre_norm"][l].reshape(8, 128).T
        cw = inp["conv_w"][l].reshape(3, 12, 128)
        colp[l, :, 8:44] = cw.transpose(2, 1, 0).reshape(128, 36)
        colp[l, :, 44:56] = inp["conv_b"][l].reshape(12, 128).T
        colp[l, :, 56] = inp["subln"][l]
        colp[l, :64, 57] = inp["flt_freq"][l]
        colp[l, :64, 58] = inp["flt_b1"][l]
        colp[l, :64, 59] = inp["flt_b2"][l]
        colp[l, :64, 60] = inp["flt_b3"][l]
        rowp[l, 0:1024] = inp["post_norm"][l]
        rowp[l, 1024:3072] = inp["flt_deltas"][l]
        rowp[l, 3072:4096] = inp["flt_bias"][l].reshape(-1)
        rowp[l, 4096:4160] = inp["lam_q1"][l]
        rowp[l, 4160:4224] = inp["lam_k1"][l]
        rowp[l, 4224:4288] = inp["lam_q2"][l]
        rowp[l, 4288:4352] = inp["lam_k2"][l]
    return colp, rowp


def build_program(nlayers=DEPTH):
    nc = bass.Bass("TRN2", target_bir_lowering=False)
    dt = nc.dram_tensor
    x_in = dt("x", [NBC, S, D], F32, kind="ExternalInput").ap()
    w_in = dt("w_in", [DEPTH, D, WCOLS], F32, kind="ExternalInput").ap()
    w_pa = dt("w_pa", [DEPTH, 512, D], F32, kind="ExternalInput").ap()
    w_ph = dt("w_ph", [DEPTH, 512, D], F32, kind="ExternalInput").ap()
    w_out = dt("w_out", [DEPTH, D, D], F32, kind="ExternalInput").ap()
    flt_w1 = dt("flt_w1", [DEPTH, 33, 64], F32, kind="ExternalInput").ap()
    flt_w2 = dt("flt_w2", [DEPTH, 64, 64], F32, kind="ExternalInput").ap()
    flt_w3 = dt("flt_w3", [DEPTH, 64, 64], F32, kind="ExternalInput").ap()
    flt_wo = dt("flt_w_out", [DEPTH, 64, 2048], F32, kind="ExternalInput").ap()
    rel_bias = dt("rel_bias", [32, 4], F32, kind="ExternalInput").ap()
    colp_d = dt("colp", [DEPTH, 128, NCOLP], F32, kind="ExternalInput").ap()
    rowp_h = dt("rowp", [DEPTH, NROWP], F32, kind="ExternalInput")
    rowp_d = rowp_h.ap()
    rowb = lambda l, a, n: bass.AP(tensor=rowp_h, offset=l * NROWP + a, ap=[[0, P], [1, n]])
    fw_d = dt("fw", [32, 128, 16, 128], BF16, kind="ExternalInput").ap()
    iv_d = dt("iv", [32, 128, 16, 128], BF16, kind="ExternalInput").ap()
    zT_d = dt("zT", [33, 2048], F32, kind="ExternalInput").ap()
    negt_d = dt("negt", [128, 17], F32, kind="ExternalInput").ap()
    oh_d = dt("onehot", [32, R_B], F32, kind="ExternalInput").ap()
    id_d = dt("ident", [128, 128], BF16, kind="ExternalInput").ap()
    y_d = dt("y", [NBC, S, D], F32, kind="ExternalOutput").ap()
    gs_h = dt("gs", [2, 16, 128, NH, 2 * CW], F32, kind="Internal")
    gs_d = gs_h.ap()
    bv_h = dt("bvec", [4, R_B], BF16, kind="Internal")

    with contextlib.ExitStack() as es:
        sc = Sched(nc, es)
        uid = [0]

        def _alloc(stack, name, shape, dty):
            uid[0] += 1
            return stack.enter_context(nc.sbuf_tensor(f"s{uid[0]}_{name}", shape, dty))

        sb = lambda name, shape, dty: _alloc(es, name, shape, dty)

        ident = sb("ident", [P, P], BF16)
        ones = sb("ones", [P, P], BF16)
        negt = sb("negt", [P, 17], F32)
        colp = sb("colp", [P, NCOLP], F32)
        misc = sb("misc", [P, 48], F32)
        TB = sb("TB", [P, 4, TBW], BF16)
        hT = sb("hT", [P, KT, S], BF16)
        yhT = sb("yhT", [P, 4, S], BF16)
        NW = 4
        Wsl = [sb(f"W{i}", [P, KT, 256], BF16) for i in range(NW)]
        b_hn2 = [Buf(), Buf()]
        b_pm = [Buf(), Buf()]
        PS = [es.enter_context(nc.psum_tensor(f"ps{i}", [P, 512], F32)) for i in range(8)]

        b_ident, b_negt, b_colp, b_misc, b_TB = Buf(), Buf(), Buf(), Buf(), Buf()
        b_hTa = [Buf() for _ in range(16)]
        b_hTb = [Buf() for _ in range(16)]
        hTr = lambda a, b: b_hTa[a:b] + b_hTb[a:b]
        b_yaT = [[Buf() for _ in range(4)] for _ in range(4)]
        b_yhT = [Buf() for _ in range(4)]
        b_W = [Buf() for _ in range(NW)]
        b_xs = [Buf(), Buf()]
        b_junk = Buf()
        b_PS = [Buf() for _ in range(8)]
        s_W = [sc.newsem(f"sW{i}") for i in range(NW)]
        s_xs = [sc.newsem(f"sxs{i}") for i in range(2)]
        s_c = sc.newsem("sconst")
        s_st = [sc.newsem(f"sst{i}") for i in range(2)]
        s_a = [sc.newsem(f"sa{i}") for i in range(8)]
        wctr = [0]

        def load_w(src_aps):
            i = wctr[0] % NW
            wctr[0] += 1
            for ap_, off, n in src_aps:
                sc.dma("pool", Wsl[i][:, :, off:off + n], ap_, s_W[i], writes=[b_W[i]])
            return i

        def win_cols(l, c0, n):
            return w_in[l].rearrange("(kt p) n -> p kt n", p=P)[:, :, c0:c0 + n]

        sc.dma("sp", ident[:], id_d[:, :], s_c, writes=[b_ident], shared=True)
        sc.dma("sp", negt[:], negt_d[:, :], s_c, writes=[b_negt], shared=True)
        sc.op("pool", lambda e: e.memset(ones[:], 1.0), writes=[b_ident])
        with contextlib.ExitStack() as ph:
            sbp = lambda name, shape, dty: _alloc(ph, name, shape, dty)
            rb = sbp("rb", [32, 4], F32)
            oh = sbp("oh", [32, R_B], F32)
            bvs = sbp("bvs", [4, R_B], BF16)
            b_rb, b_oh, b_bvs = Buf(), Buf(), Buf()
            sc.dma("sp", rb[:], rel_bias[:, :], s_c, writes=[b_rb], shared=True)
            sc.dma("sp", oh[:], oh_d[:, :], s_c, writes=[b_oh], shared=True)
            sc.flush(s_c)
            for j, (c0, n) in enumerate([(0, 512), (512, 512), (1024, 256)]):
                sc.group("pe", [lambda e, c0=c0, n=n, j=j: e.matmul(PS[j][0:4, 0:n], rb[:, :], oh[:, c0:c0 + n],
                                                                    start=True, stop=True)],
                         reads=[b_rb, b_oh], writes=[b_PS[j]])
                sc.op("dve", lambda e, c0=c0, n=n, j=j: e.tensor_copy(out=bvs[:, c0:c0 + n], in_=PS[j][0:4, 0:n]),
                      reads=[b_PS[j]], writes=[b_bvs])
            sc.dma("sp", bv_h.ap()[:, :], bvs[:], s_c, reads=[b_bvs], shared=True)
            sc.flush(s_c)
            sc.barrier()
            for p in range(P):
                src = bass.AP(tensor=bv_h, offset=127 - p, ap=[[0, 1], [R_B, 4], [1, TBW]])
                sc.dma("sp", TB[p:p + 1, :, :], src, s_c, writes=[b_TB], shared=True)
            sc.flush(s_c)
            sc.barrier()
            for h in range(4):
                sc.op("dve", lambda e, h=h: e.tensor_copy(out=misc[:, 22 + 2 * h:23 + 2 * h], in_=TB[:, h, 0:1]),
                      reads=[b_TB], writes=[b_misc])
                sc.op("dve", lambda e, h=h: e.tensor_copy(out=misc[:, 23 + 2 * h:24 + 2 * h], in_=TB[:, h, TBW - 1:TBW]),
                      reads=[b_TB], writes=[b_misc])

        for l in range(nlayers):
            lam_init = 0.8 - 0.6 * math.exp(-0.3 * l)
            sc.dma("sp", colp[:], colp_d[l], s_c, writes=[b_colp], shared=True)
            with contextlib.ExitStack() as ph:
                sbp = lambda name, shape, dty: _alloc(ph, name, shape, dty)
                lamv = sbp("lamv", [P, 256], F32)
                lamt = sbp("lamt", [P, 128], F32)
                b_lv, b_lt = Buf(), Buf()
                sc.dma("sp", lamv[:], rowb(l, 4096, 256), s_c, writes=[b_lv], shared=True)
                sc.flush(s_c)
                sc.op("dve", lambda e: e.tensor_tensor(out=lamt[:, 0:64], in0=lamv[:, 0:64], in1=lamv[:, 64:128], op=ALU.mult),
                      reads=[b_lv], writes=[b_lt])
                sc.op("dve", lambda e: e.tensor_tensor(out=lamt[:, 64:128], in0=lamv[:, 128:192], in1=lamv[:, 192:256], op=ALU.mult),
                      reads=[b_lv], writes=[b_lt])
                sc.op("dve", lambda e: e.reduce_sum(out=misc[:, 6:7], in_=lamt[:, 0:64], axis=AX.X), reads=[b_lt], writes=[b_misc])
                sc.op("dve", lambda e: e.reduce_sum(out=misc[:, 7:8], in_=lamt[:, 64:128], axis=AX.X), reads=[b_lt], writes=[b_misc])
                sc.op("act", lambda e: e.activation(out=misc[:, 8:10], in_=misc[:, 6:8], func=AF.Exp), reads=[b_misc], writes=[b_misc])
                sc.op("dve", lambda e: e.tensor_tensor(out=misc[:, 10:11], in0=misc[:, 9:10], in1=misc[:, 8:9], op=ALU.subtract),
                      reads=[b_misc], writes=[b_misc])
                sc.op("dve", lambda e: e.tensor_scalar(out=misc[:, 0:1], in0=misc[:, 10:11], scalar1=-lam_init, scalar2=None, op0=ALU.add),
                      reads=[b_misc], writes=[b_misc])
                sc.op("dve", lambda e: e.tensor_scalar(out=misc[:, 1:2], in0=colp[:, 56:57], scalar1=(1.0 - lam_init), scalar2=None, op0=ALU.mult),
                      reads=[b_colp, b_misc], writes=[b_misc])
                sc.op("dve", lambda e: e.tensor_scalar(out=misc[:, 2:3], in0=colp[:, 57:58], scalar1=1.0 / TWO_PI, scalar2=None, op0=ALU.mult),
                      reads=[b_colp, b_misc], writes=[b_misc])
                for j in range(3):
                    sc.op("dve", lambda e, j=j: e.tensor_scalar(out=misc[:, 3 + j:4 + j], in0=colp[:, 58 + j:59 + j],
                                                              scalar1=misc[:, 2:3], scalar2=16.0, op0=ALU.mult, op1=ALU.add),
                          reads=[b_colp, b_misc], writes=[b_misc])
                sc.barrier()

            with contextlib.ExitStack() as ph:
                sbp = lambda name, shape, dty: _alloc(ph, name, shape, dty)
                aA = sbp("aA", [64, S], F32)
                wo = sbp("wo", [64, 2048], F32)
                dabs = sbp("dabs", [P, 2048], F32)
                brow = sbp("brow", [1, 1024], F32)
                a3b = sbp("a3b", [64, S], BF16)
                wob = sbp("wob", [64, 2048], BF16)
                b_a3b, b_wob = Buf(), Buf()
                b_aA, b_wo, b_dabs, b_brow = Buf(), Buf(), Buf(), Buf()
                sc.dma("sp", wo[:], flt_wo[l], s_c, writes=[b_wo], shared=True)
                sc.dma("sp", dabs[:], rowb(l, 1024, 2048), s_c, writes=[b_dabs], shared=True)
                sc.dma("sp", brow[:], rowp_d[l:l + 1, 3072:4096], s_c, writes=[b_brow], shared=True)
                sc.flush(s_c)
                sc.op("act", lambda e: e.activation(out=dabs[:], in_=dabs[:], func=AF.Abs), reads=[b_dabs], writes=[b_dabs])
                with contextlib.ExitStack() as ph2:
                    sbq = lambda name, shape, dty: _alloc(ph2, name, shape, dty)
                    zT = sbq("zT", [33, S], F32)
                    aB = sbq("aB", [64, S], F32)
                    w1 = sbq("w1", [33, 64], F32)
                    w2 = sbq("w2", [64, 64], F32)
                    w3 = sbq("w3", [64, 64], F32)
                    tA = [sbq(f"tA{i}", [64, 512], F32) for i in range(2)]
                    tI = [sbq(f"tI{i}", [64, 512], I32) for i in range(2)]
                    tF = [sbq(f"tF{i}", [64, 512], F32) for i in range(2)]
                    b_zT, b_aB, b_w = Buf(), Buf(), Buf()
                    b_tA, b_tI, b_tF = [Buf(), Buf()], [Buf(), Buf()], [Buf(), Buf()]
                    sc.dma("sp", zT[:], zT_d[:, :], s_c, writes=[b_zT], shared=True)
                    sc.dma("sp", w1[:], flt_w1[l], s_c, writes=[b_w], shared=True)
                    sc.dma("sp", w2[:], flt_w2[l], s_c, writes=[b_w], shared=True)
                    sc.dma("sp", w3[:], flt_w3[l], s_c, writes=[b_w], shared=True)
                    sc.flush(s_c)
                    chain = [(w1, 33, zT, b_zT, aA, b_aA), (w2, 64, aA, b_aA, aB, b_aB), (w3, 64, aB, b_aB, aA, b_aA)]
                    it = 0
                    for j, (wj, kk, src, b_src, dst, b_dst) in enumerate(chain):
                        for ch in range(4):
                            sl = slice(ch * 512, (ch + 1) * 512)
                            k = it % 2
                            pb = it % 2
                            it += 1
                            sc.group("pe", [lambda e, wj=wj, kk=kk, src=src, sl=sl, pb=pb: e.matmul(
                                PS[pb][0:64, :], wj[0:kk, :], src[0:kk, sl], start=True, stop=True)],
                                reads=[b_w, b_src], writes=[b_PS[pb]])
                            sc.op("dve", lambda e, k=k, pb=pb, j=j: e.tensor_scalar(
                                out=tA[k][:], in0=PS[pb][0:64, :], scalar1=misc[0:64, 2:3], scalar2=misc[0:64, 3 + j:4 + j],
                                op0=ALU.mult, op1=ALU.add), reads=[b_PS[pb], b_misc], writes=[b_tA[k]])
                            sc.op("dve", lambda e, k=k: e.tensor_copy(out=tI[k][:], in_=tA[k][:]), reads=[b_tA[k]], writes=[b_tI[k]])
                            sc.op("dve", lambda e, k=k: e.tensor_copy(out=tF[k][:], in_=tI[k][:]), reads=[b_tI[k]], writes=[b_tF[k]])
                            sc.op("dve", lambda e, k=k: e.tensor_tensor(out=tA[k][:], in0=tA[k][:], in1=tF[k][:], op=ALU.subtract),
                                  reads=[b_tA[k], b_tF[k]], writes=[b_tA[k]])
                            sc.op("dve", lambda e, k=k: e.tensor_scalar(out=tA[k][:], in0=tA[k][:], scalar1=0.5, scalar2=-0.5,
                                                                      op0=ALU.min, op1=ALU.max),
                                  reads=[b_tA[k]], writes=[b_tA[k]])
                            sc.op("act", lambda e, k=k, dst=dst, sl=sl: e.activation(out=dst[:, sl], in_=tA[k][:], func=AF.Sin, scale=TWO_PI),
                                  reads=[b_tA[k]], writes=[b_dst])
                    sc.op("dve", lambda e: e.tensor_copy(out=a3b[:], in_=aA[:]), reads=[b_aA], writes=[b_a3b])
                    sc.op("pool", lambda e: e.tensor_copy(out=wob[:], in_=wo[:]), reads=[b_wo], writes=[b_wob])
                    sc.barrier()
                with contextlib.ExitStack() as ph2:
                    sbq = lambda name, shape, dty: _alloc(ph2, name, shape, dty)
                    FS = sbq("FS", [P, 16, NH, 2 * CW], BF16)
                    FD = sbq("FD", [P, 16, NH, 2 * CW], BF16)
                    dec = [sbq(f"dec{i}", [P, 512], F32) for i in range(2)]
                    hfb = [sbq(f"hfb{i}", [P, 512], F32) for i in range(2)]
                    FWs = [sbq(f"FWs{i}", [P, 16, 128], BF16) for i in range(3)]
                    Gst = [sbq(f"Gst{i}", [P, 2 * CW], F32) for i in range(2)]
                    b_FS, b_FD = [Buf() for _ in range(16)], [Buf() for _ in range(16)]
                    b_dec, b_hfb = [Buf(), Buf()], [Buf(), Buf()]
                    b_FWs, b_Gst = [Buf() for _ in range(3)], [Buf(), Buf()]
                    gc = 0
                    sgn = negt[:, 16:17]
                    for o in range(2):
                        for tt in range(16):
                            for di in range(2):
                                cs = slice(di * 1024 + o * 512, di * 1024 + (o + 1) * 512)
                                pb = di
                                sc.group("pe", [lambda e, tt=tt, cs=cs, pb=pb: e.matmul(
                                    PS[pb][:, :], a3b[:, tt * 128:(tt + 1) * 128], wob[:, cs], start=True, stop=True)],
                                    reads=[b_a3b, b_wob], writes=[b_PS[pb]])
                                sc.op("act", lambda e, di=di, cs=cs, tt=tt: e.activation(
                                    out=dec[di][:], in_=dabs[:, cs], func=AF.Exp, scale=negt[:, tt:tt + 1]),
                                    reads=[b_dabs, b_negt], writes=[b_dec[di]])
                                sc.op("dve", lambda e, di=di, pb=pb: e.tensor_tensor(out=hfb[di][:], in0=PS[pb][:, :], in1=dec[di][:], op=ALU.mult),
                                      reads=[b_PS[pb], b_dec[di]], writes=[b_hfb[di]])
                            if tt == 0:
                                sc.op("dve", lambda e: e.memset(hfb[1][0:1, :], 0.0), writes=[b_hfb[1]])
                                sc.op("dve", lambda e, o=o: e.tensor_tensor(out=hfb[0][0:1, :], in0=hfb[0][0:1, :],
                                                                          in1=brow[0:1, o * 512:(o + 1) * 512], op=ALU.add),
                                      reads=[b_brow], writes=[b_hfb[0]])
                            h0 = hfb[0][:, :].rearrange("p (h c) -> p h c", h=NH)
                            h1 = hfb[1][:, :].rearrange("p (h c) -> p h c", h=NH)
                            sc.op("pool", lambda e, tt=tt, h0=h0, h1=h1: e.tensor_tensor(out=FS[:, tt, :, 0:CW], in0=h0, in1=h1, op=ALU.add),
                                  reads=[b_hfb[0], b_hfb[1]], writes=[b_FS[tt]])
                            sc.op("pool", lambda e, tt=tt, h0=h0, h1=h1: e.tensor_tensor(out=FD[:, tt, :, 0:CW], in0=h0, in1=h1, op=ALU.subtract),
                                  reads=[b_hfb[0], b_hfb[1]], writes=[b_FD[tt]])
                            sc.op("act", lambda e, tt=tt: e.activation(out=FS[:, tt, :, CW:2 * CW], in_=FS[:, tt, :, 0:CW], func=AF.Identity, scale=sgn),
                                  reads=[b_negt], writes=[b_FS[tt]])
                            sc.op("act", lambda e, tt=tt: e.activation(out=FD[:, tt, :, CW:2 * CW], in_=FD[:, tt, :, 0:CW], func=AF.Identity, scale=sgn),
                                  reads=[b_negt], writes=[b_FD[tt]])
                        for hf in range(NH):

                            def fw_load(ct):
                                fi = ct % 3
                                sc.dma("sp", FWs[fi][:], fw_d[ct if ct < 8 else ct + 8], s_a[fi], writes=[b_FWs[fi]])
                            fw_load(0)
                            fw_load(1)
                            for ct in range(16):
                                fi = ct % 3
                                src, b_src = (FS, b_FS) if ct < 8 else (FD, b_FD)
                                pb = 2 + (ct % 2)
                                sc.group("pe", [lambda e, st=st, fi=fi, src=src, pb=pb: e.matmul(
                                    PS[pb][:, :], FWs[fi][:, st, :], src[:, st, hf, :], start=(st == 0), stop=(st == 15)) for st in range(16)],
                                    reads=[b_FWs[fi]] + b_src, writes=[b_PS[pb]])
                                gi = gc % 2
                                gc += 1
                                sc.op("act", lambda e, gi=gi, pb=pb: e.activation(out=Gst[gi][:], in_=PS[pb][:, :], func=AF.Copy),
                                      reads=[b_PS[pb]], writes=[b_Gst[gi]])
                                if ct + 2 < 16:
                                    fw_load(ct + 2)
                                sc.dma("sp", gs_d[o, ct, :, hf, :], Gst[gi][:], s_a[3 + gi], reads=[b_Gst[gi]])
                    sc.barrier()

            for b in range(NBC):
                xsrc = x_in[b] if l == 0 else y_d[b]
                pa_scope = contextlib.ExitStack()
                xs = [_alloc(pa_scope, f"xs{i}", [P, D], F32) for i in range(2)]
                hn2 = [_alloc(pa_scope, f"hn{i}", [P, D], BF16) for i in range(2)]
                junk = _alloc(pa_scope, "junk", [P, D], BF16)
                def pa_stage1(tt):
                    k = tt % 2
                    c0 = 12 + 3 * k
                    bm = b_pm[k]
                    sc.dma("sp", xs[k][:], xsrc[tt * 128:(tt + 1) * 128, :], s_xs[k], writes=[b_xs[k]])
                    sc.op("dve", lambda e: e.memset(misc[:, c0:c0 + 1], 0.0), writes=[bm])
                    sc.op("act", lambda e: e.activation(out=junk[:], in_=xs[k][:], func=AF.Square, accum_out=misc[:, c0:c0 + 1]),
                          reads=[b_xs[k]], writes=[b_junk, bm])
                    sc.op("act", lambda e: e.activation(out=misc[:, c0 + 1:c0 + 2], in_=misc[:, c0:c0 + 1], func=AF.Ln, scale=1.0 / D, bias=NORM_EPS),
                          reads=[bm], writes=[bm])
                    sc.op("act", lambda e: e.activation(out=misc[:, c0 + 2:c0 + 3], in_=misc[:, c0 + 1:c0 + 2], func=AF.Exp, scale=-0.5),
                          reads=[bm], writes=[bm])
                    sc.op("act", lambda e: e.activation(out=hn2[k][:], in_=xs[k][:], func=AF.Identity, scale=misc[:, c0 + 2:c0 + 3]),
                          reads=[b_xs[k], bm], writes=[b_hn2[k]])

                def pa_stage2(tt):
                    k = tt % 2
                    pb = 6 + (tt % 2)
                    pT = PS[pb][:, :].bitcast(BF16)
                    sc.group("pe", [lambda e, kt=kt: e.transpose(out=pT[:, kt * 128:(kt + 1) * 128],
                                                                 in_=hn2[k][:, kt * 128:(kt + 1) * 128], identity=ident[:])
                                    for kt in range(KT)], reads=[b_hn2[k], b_ident], writes=[b_PS[pb]])
                    for kt in range(KT):
                        sc.op("dve", lambda e, kt=kt: e.tensor_scalar(
                            out=hT[:, kt, tt * 128:(tt + 1) * 128], in0=pT[:, kt * 128:(kt + 1) * 128],
                            scalar1=colp[:, kt:kt + 1], scalar2=None, op0=ALU.mult),
                            reads=[b_PS[pb], b_colp], writes=[b_hTa[tt]])

                pa_stage1(0)
                for tt in range(16):
                    if tt + 1 < 16:
                        pa_stage1(tt + 1)
                    pa_stage2(tt)
                sc.barrier()
                pa_scope.close()

                for hf in range(NH):
                    with contextlib.ExitStack() as ph:
                        sbp = lambda name, shape, dty: _alloc(ph, name, shape, dty)
                        NCT = CW // 128
                        ust = [sbp(f"ust{i}", [P, S + 2], F32) for i in range(2)]
                        uc = [sbp(f"uc{i}", [P, S], BF16) for i in range(2)]
                        U = sbp("U", [P, 16, 2 * CW], BF16)
                        HX1 = sbp("HX1", [P, 16, CW], BF16)
                        G2 = sbp("G2", [P, 16, CW], BF16)
                        Y = sbp("Y", [P, 32 * CW], BF16)
                        DS = [sbp(f"DS{i}", [P, 16, 128], BF16) for i in range(4)]
                        Gsl = [sbp(f"Gsl{i}", [P, 2, 2 * CW], F32) for i in range(2)]
                        cm = [sbp(f"cm{i}", [P, 4, 2 * CW], F32) for i in range(2)]
                        yht = [sbp(f"yht{i}", [P, CW], BF16) for i in range(2)]
                        tA_ = [sbp(f"tA_{i}", [P, CW], F32) for i in range(2)]
                        yv = [sbp(f"yv{i}", [P, CW], F32) for i in range(2)]
                        b_tA_, b_yv = [Buf(), Buf()], [Buf(), Buf()]
                        sgn = negt[:, 16:17]
                        b_ust, b_uc = [Buf(), Buf()], [Buf(), Buf()]
                        b_U, b_HX1, b_G2 = [Buf() for _ in range(16)], [Buf() for _ in range(16)], [Buf() for _ in range(16)]
                        b_Y = [Buf() for _ in range(32)]
                        b_DS = [Buf() for _ in range(4)]
                        b_Gsl, b_cm, b_yht = [Buf(), Buf()], [Buf(), Buf()], [Buf(), Buf()]
                        for i in range(2):
                            sc.op("pool", lambda e, i=i: e.memset(ust[i][:, 0:1], 0.0), writes=[b_ust[i]])
                            sc.op("pool", lambda e, i=i: e.memset(ust[i][:, S + 1:S + 2], 0.0), writes=[b_ust[i]])
                        ui = 0
                        tpc = 0
                        for grp, (cbase, dstT, b_dst) in enumerate([(2048, U, b_U), (2560, HX1, b_HX1), (3072, G2, b_G2), (3584, G2, b_G2)]):
                            wi = load_w([(win_cols(l, cbase + hf * CW, CW), 0, CW)])
                            for j in range(NCT):
                                k = ui % 2
                                ui += 1
                                ctg = (cbase - 2048) // 128 + hf * NCT + j
                                for tc in range(4):
                                    pb = tc % 2
                                    sc.group("pe", [lambda e, kt=kt, wi=wi, j=j, tc=tc, pb=pb: e.matmul(
                                        PS[pb][:, :], Wsl[wi][:, kt, j * 128:(j + 1) * 128], hT[:, kt, tc * 512:(tc + 1) * 512],
                                        start=(kt == 0), stop=(kt == KT - 1)) for kt in range(KT)],
                                        reads=[b_W[wi]] + hTr(tc * 4, (tc + 1) * 4), writes=[b_PS[pb]])
                                    if grp < 3:
                                        sc.op("act", lambda e, k=k, tc=tc, pb=pb: e.activation(
                                            out=ust[k][:, 1 + tc * 512:1 + (tc + 1) * 512], in_=PS[pb][:, :], func=AF.Copy),
                                            reads=[b_PS[pb]], writes=[b_ust[k]])
                                    else:
                                        sc.op("act", lambda e, k=k, tc=tc, pb=pb: e.activation(
                                            out=uc[k][:, tc * 512:(tc + 1) * 512], in_=PS[pb][:, :], func=AF.Silu),
                                            reads=[b_PS[pb]], writes=[b_uc[k]])
                                if grp < 3:
                                    c0 = 8 + ctg * 3
                                    acc = Y[:, 0:2 * S].bitcast(F32)
                                    sc.op("act", lambda e, k=k, c0=c0, ctg=ctg, acc=acc: e.activation(
                                        out=acc[:, 0:S], in_=ust[k][:, 0:S], func=AF.Identity, scale=colp[:, c0:c0 + 1],
                                        bias=colp[:, 44 + ctg:45 + ctg]), reads=[b_ust[k], b_colp], writes=[b_cm[0]])
                                    sc.op("dve", lambda e, k=k, c0=c0, acc=acc: e.scalar_tensor_tensor(
                                        out=acc[:, 0:S], in0=ust[k][:, 1:S + 1], scalar=colp[:, c0 + 1:c0 + 2], in1=acc[:, 0:S],
                                        op0=ALU.mult, op1=ALU.add), reads=[b_ust[k], b_colp, b_cm[0]], writes=[b_cm[0]])
                                    sc.op("dve", lambda e, k=k, c0=c0, acc=acc: e.scalar_tensor_tensor(
                                        out=uc[k][:, :], in0=ust[k][:, 2:S + 2], scalar=colp[:, c0 + 2:c0 + 3], in1=acc[:, 0:S],
                                        op0=ALU.mult, op1=ALU.add), reads=[b_ust[k], b_colp, b_cm[0]], writes=[b_uc[k]])
                                for g8 in range(2):
                                    pb = 6 + (tpc % 2)
                                    tpc += 1
                                    pT = PS[pb][:, :].bitcast(BF16)
                                    sc.group("pe", [lambda e, k=k, i=i, g8=g8, pT=pT: e.transpose(
                                        out=pT[:, i * 128:(i + 1) * 128], in_=uc[k][:, (g8 * 8 + i) * 128:(g8 * 8 + i + 1) * 128],
                                        identity=ident[:]) for i in range(8)], reads=[b_uc[k], b_ident], writes=[b_PS[pb]])
                                    dsl = dstT[:, g8 * 8:(g8 + 1) * 8, j * 128:(j + 1) * 128]
                                    srcv = pT.rearrange("p (a b) -> p a b", a=8)
                                    if grp < 3:
                                        sc.op("act", lambda e, dsl=dsl, srcv=srcv: e.activation(out=dsl, in_=srcv, func=AF.Copy),
                                              reads=[b_PS[pb]], writes=b_dst[g8 * 8:(g8 + 1) * 8])
                                        if grp == 0:
                                            dsm = dstT[:, g8 * 8:(g8 + 1) * 8, CW + j * 128:CW + (j + 1) * 128]
                                            sc.op("act", lambda e, dsl=dsl, dsm=dsm: e.activation(out=dsm, in_=dsl, func=AF.Identity, scale=sgn),
                                                  reads=[b_negt], writes=b_dst[g8 * 8:(g8 + 1) * 8])
                                    else:
                                        sc.op("dve", lambda e, dsl=dsl, srcv=srcv: e.tensor_tensor(out=dsl, in0=srcv, in1=dsl, op=ALU.mult),
                                              reads=[b_PS[pb]], writes=b_dst[g8 * 8:(g8 + 1) * 8])
                        dsc = 0
                        gsc = 0
                        cmc = 0
                        N2 = 2 * CW
                        pend_tr = []
                        for o in range(2):
                            for fp in range(8):
                                d0 = dsc % 4
                                d1 = (dsc + 1) % 4
                                dsc += 2
                                sc.dma("sp", DS[d0][:], fw_d[fp], s_a[d0], writes=[b_DS[d0]])
                                sc.dma("sp", DS[d1][:], fw_d[fp + 16], s_a[d1], writes=[b_DS[d1]])
                                gi = gsc % 2
                                gsc += 1
                                sc.dma("sp", Gsl[gi][:, 0, :], gs_d[o, fp, :, hf, :], s_a[4 + gi], writes=[b_Gsl[gi]])
                                sc.dma("sp", Gsl[gi][:, 1, :], gs_d[o, fp + 8, :, hf, :], s_a[4 + gi], writes=[b_Gsl[gi]])
                                for half, dd in ((0, d0), (1, d1)):
                                    pb = 2 + half + 2 * (fp % 2)
                                    sc.group("pe", [lambda e, st=st, dd=dd, pb=pb: e.matmul(
                                        PS[pb][:, 0:N2], DS[dd][:, st, :], U[:, st, :], start=(st == 0), stop=(st == 15)) for st in range(16)],
                                        reads=[b_DS[dd]] + b_U, writes=[b_PS[pb]])
                                pr = 2 + 2 * (fp % 2)
                                pi_ = pr + 1
                                ci = cmc % 2
                                cmc += 1
                                c = cm[ci]
                                sc.op("dve", lambda e, c=c, pr=pr, gi=gi: e.tensor_tensor(out=c[:, 0, :], in0=PS[pr][:, 0:N2], in1=Gsl[gi][:, 0, :], op=ALU.mult),
                                      reads=[b_PS[pr], b_Gsl[gi]], writes=[b_cm[ci]])
                                sc.op("dve", lambda e, c=c, pi_=pi_, gi=gi: e.tensor_tensor(out=c[:, 1, :], in0=PS[pi_][:, 0:N2], in1=Gsl[gi][:, 1, :], op=ALU.mult),
                                      reads=[b_PS[pi_], b_Gsl[gi]], writes=[b_cm[ci]])
                                sc.op("dve", lambda e, c=c, pr=pr, gi=gi: e.tensor_tensor(out=c[:, 2, :], in0=PS[pr][:, 0:N2], in1=Gsl[gi][:, 1, :], op=ALU.mult),
                                      reads=[b_PS[pr], b_Gsl[gi]], writes=[b_cm[ci]])
                                sc.op("dve", lambda e, c=c, pi_=pi_, gi=gi: e.tensor_tensor(out=c[:, 3, :], in0=PS[pi_][:, 0:N2], in1=Gsl[gi][:, 0, :], op=ALU.mult),
                                      reads=[b_PS[pi_], b_Gsl[gi]], writes=[b_cm[ci]])
                                sc.op("pool", lambda e, c=c, fp=fp: e.tensor_tensor(out=Y[:, fp * N2:(fp + 1) * N2], in0=c[:, 0, :], in1=c[:, 1, :], op=ALU.subtract),
                                      reads=[b_cm[ci]], writes=[b_Y[fp]])
                                sc.op("pool", lambda e, c=c, fp=fp: e.tensor_tensor(out=Y[:, (fp + 8) * N2:(fp + 9) * N2], in0=c[:, 2, :], in1=c[:, 3, :], op=ALU.add),
                                      reads=[b_cm[ci]], writes=[b_Y[fp + 8]])
                            for tt in range(16):
                                d0 = dsc % 4
                                d1 = (dsc + 1) % 4
                                dsc += 2
                                sc.dma("sp", DS[d0][:, 0:8, :], iv_d[tt * 2, :, 0:8, :], s_a[d0], writes=[b_DS[d0]])
                                sc.dma("sp", DS[d1][:, 0:8, :], iv_d[tt * 2 + 1, :, 0:8, :], s_a[d1], writes=[b_DS[d1]])
                                pb = 2 + (tt % 2)
                                fns = []
                                for hh, dd in ((0, d0), (1, d1)):
                                    for c_ in range(8):
                                        ct = hh * 8 + c_
                                        fns.append(lambda e, dd=dd, c_=c_, ct=ct, pb=pb: e.matmul(
                                            PS[pb][:, 0:N2], DS[dd][:, c_, :], Y[:, ct * N2:(ct + 1) * N2], start=(ct == 0), stop=(ct == 15)))
                                sc.group("pe", fns, reads=[b_DS[d0], b_DS[d1]] + b_Y[0:16], writes=[b_PS[pb]])
                                k = tt % 2
                                sc.op("act", lambda e, k=k, pb=pb: e.activation(out=tA_[k][:], in_=PS[pb][:, 0:CW], func=AF.Copy),
                                      reads=[b_PS[pb]], writes=[b_tA_[k]])
                                sc.op("dve", lambda e, k=k, pb=pb: e.scalar_tensor_tensor(
                                    out=yv[k][:], in0=PS[pb][:, CW:N2], scalar=sgn, in1=tA_[k][:], op0=ALU.mult, op1=ALU.add),
                                    reads=[b_PS[pb], b_tA_[k], b_negt], writes=[b_yv[k]])
                                if o == 0:
                                    sc.op("dve", lambda e, tt=tt, k=k: e.tensor_tensor(out=U[:, tt, 0:CW], in0=yv[k][:], in1=HX1[:, tt, :], op=ALU.mult),
                                          reads=[b_yv[k], b_HX1[tt]], writes=[b_U[tt]])
                                    sc.op("act", lambda e, tt=tt: e.activation(out=U[:, tt, CW:N2], in_=U[:, tt, 0:CW], func=AF.Identity, scale=sgn),
                                          reads=[b_negt], writes=[b_U[tt]])
                                else:
                                    sc.op("dve", lambda e, tt=tt, k=k: e.tensor_tensor(out=yht[k][:], in0=yv[k][:], in1=G2[:, tt, :], op=ALU.mult),
                                          reads=[b_yv[k], b_G2[tt]], writes=[b_yht[k]])
                                    def yh_tr(tt=tt, k=k):
                                        pq = 6 + (tt % 2)
                                        pT = PS[pq][:, :].bitcast(BF16)
                                        sc.group("pe", [lambda e, j=j: e.transpose(
                                            out=pT[:, j * 128:(j + 1) * 128], in_=yht[k][:, j * 128:(j + 1) * 128], identity=ident[:])
                                            for j in range(NCT)], reads=[b_yht[k], b_ident], writes=[b_PS[pq]])
                                        for j in range(NCT):
                                            cti = hf * NCT + j
                                            sc.op("act", lambda e, j=j, cti=cti: e.activation(
                                                out=yhT[:, cti, tt * 128:(tt + 1) * 128], in_=pT[:, j * 128:(j + 1) * 128], func=AF.Copy),
                                                reads=[b_PS[pq]], writes=[b_yhT[cti]])
                                    if pend_tr:
                                        pend_tr.pop(0)()
                                    pend_tr.append(yh_tr)
                            while pend_tr:
                                pend_tr.pop(0)()
                        sc.barrier()

                bs_scope = contextlib.ExitStack()
                yaT = _alloc(bs_scope, "yaT", [P, 4, S], BF16)
                with contextlib.ExitStack() as ph:
                    sbp = lambda name, shape, dty: _alloc(ph, name, shape, dty)
                    qT = sbp("qT", [P, S], BF16)
                    kTt = sbp("kTt", [P, S], BF16)
                    V = sbp("V", [P, 16, 128], BF16)
                    sza = sbp("sza", [P, S], BF16)
                    Es = [sbp(f"Es{i}", [P, 512], BF16) for i in range(4)]
                    tmpb = [sbp(f"tmpb{i}", [P, 512], F32) for i in range(2)]
                    cb = [sbp(f"cb{i}", [P, 512], F32) for i in range(5)]
                    sqs = [sbp(f"sq{i}", [P, 512], BF16) for i in range(2)]
                    cbo = [sbp(f"cbo{i}", [P, 512], F32) for i in range(2)]
                    b_sqs, b_cbo = [Buf(), Buf()], [Buf(), Buf()]
                    ecs = [0, 0, 0]
                    Ez = [sbp(f"Ez{i}", [P, 512], BF16) for i in range(2)]
                    b_Ez = [Buf(), Buf()]
                    b_qT, b_kT, b_V, b_sza = Buf(), Buf(), Buf(), Buf()
                    b_Es, b_tmpb = [Buf() for _ in range(4)], [Buf(), Buf()]
                    b_cb = [Buf() for _ in range(5)]
                    ec = 0
                    tc_ = 0
                    AW = [sbp(f"AW{i}", [P, KT, 256], BF16) for i in range(8)]
                    b_AW = [Buf() for _ in range(8)]
                    for hp in range(2):
                        for g in range(4):
                            i = hp * 4 + g
                            sc.dma("pool", AW[i][:], win_cols(l, g * 512 + hp * 256, 256), s_a[i], writes=[b_AW[i]])
                    for h in range(4):
                        hp, hoff = h // 2, (h % 2) * 128
                        for (wi, dst, b_dst, fn) in ((hp * 4 + 0, qT, b_qT, AF.Copy), (hp * 4 + 1, kTt, b_kT, AF.Copy), (hp * 4 + 3, sza, b_sza, AF.Silu)):
                            for tc in range(4):
                                pb = 6 + (tc % 2)
                                sc.group("pe", [lambda e, kt=kt, wi=wi, tc=tc, pb=pb: e.matmul(
                                    PS[pb][:, :], AW[wi][:, kt, hoff:hoff + 128], hT[:, kt, tc * 512:(tc + 1) * 512],
                                    start=(kt == 0), stop=(kt == KT - 1)) for kt in range(KT)],
                                    reads=[b_AW[wi]] + hTr(tc * 4, (tc + 1) * 4), writes=[b_PS[pb]])
                                sc.op("act", lambda e, dst=dst, tc=tc, pb=pb, fn=fn: e.activation(
                                    out=dst[:, tc * 512:(tc + 1) * 512], in_=PS[pb][:, :], func=fn),
                                    reads=[b_PS[pb]], writes=[b_dst])
                        for t4 in range(4):
                            pb = 6 + (t4 % 2)
                            fns = []
                            for i in range(4):
                                tt = t4 * 4 + i
                                for kt in range(KT):
                                    fns.append(lambda e, kt=kt, tt=tt, i=i, pb=pb: e.matmul(
                                        PS[pb][:, i * 128:(i + 1) * 128], hT[:, kt, tt * 128:(tt + 1) * 128], AW[hp * 4 + 2][:, kt, hoff:hoff + 128],
                                        start=(kt == 0), stop=(kt == KT - 1)))
                            sc.group("pe", fns, reads=[b_AW[hp * 4 + 2]] + hTr(t4 * 4, (t4 + 1) * 4), writes=[b_PS[pb]])
                            sc.op("dve", lambda e, t4=t4, pb=pb: e.tensor_copy(
                                out=V[:, t4 * 4:(t4 + 1) * 4, :], in_=PS[pb][:, :].rearrange("p (a b) -> p a b", a=4)),
                                reads=[b_PS[pb]], writes=[b_V])
                        SB = [0, 1, 6, 7]
                        pairs = [(qc, c, kp) for qc in range(4) for c in range(2) for kp in range(8)]
                        pend = []
                        deferred = []

                        def emit_front(qc, c, kp, h=h):
                            n = ecs[0]
                            ecs[0] += 1
                            banks = [SB[(2 * n) % 4], SB[(2 * n + 1) % 4]]
                            eis = [(2 * n) % 4, (2 * n + 1) % 4]
                            kts = [2 * kp, 2 * kp + 1]
                            sc.group("pe", [lambda e, kt=kt, pb=pb: e.matmul(
                                PS[pb][:, :], kTt[c * 64:(c + 1) * 64, kt * 128:(kt + 1) * 128],
                                qT[c * 64:(c + 1) * 64, qc * 512:(qc + 1) * 512], start=True, stop=True)
                                for kt, pb in zip(kts, banks)],
                                reads=[b_kT, b_qT], writes=[b_PS[banks[0]], b_PS[banks[1]]])
                            for kt, ps_, ei in zip(kts, banks, eis):
                                dlt = 128 * kt - 512 * qc
                                if dlt >= 640 or dlt <= -256:
                                    col = 22 + 2 * h + (0 if dlt >= 640 else 1)
                                    sc.op("act", lambda e: e.activation(
                                        out=Es[ei][:], in_=PS[ps_][:, :], func=AF.Exp, scale=0.125, bias=misc[:, col:col + 1]),
                                        reads=[b_PS[ps_], b_misc], writes=[b_Es[ei]])
                                else:
                                    ti = ecs[1] % 2
                                    ecs[1] += 1
                                    sc.op("dve", lambda e: e.scalar_tensor_tensor(
                                        out=tmpb[ti][:], in0=PS[ps_][:, :], scalar=0.125, in1=TB[:, h, 512 - dlt:1024 - dlt],
                                        op0=ALU.mult, op1=ALU.add), reads=[b_PS[ps_], b_TB], writes=[b_tmpb[ti]])
                                    sc.op("act", lambda e: e.activation(out=Es[ei][:], in_=tmpb[ti][:], func=AF.Exp),
                                          reads=[b_tmpb[ti]], writes=[b_Es[ei]])
                            return eis

                        def emit_back(qc, c, kp, eis, h=h):
                            pv, pz = 2 + 2 * c, 3 + 2 * c
                            fns = []
                            for kt, ei in zip([2 * kp, 2 * kp + 1], eis):
                                fns.append(lambda e, kt=kt, ei=ei: e.matmul(PS[pv][:, :], V[:, kt, :], Es[ei][:], start=(kt == 0), stop=(kt == 15)))
                                fns.append(lambda e, kt=kt, ei=ei: e.matmul(PS[pz][:, :], ones[:], Es[ei][:], start=(kt == 0), stop=(kt == 15)))
                            sc.group("pe", fns, reads=[b_V, b_Es[eis[0]], b_Es[eis[1]], b_ident], writes=[b_PS[pv], b_PS[pz]])
                            if kp == 7:
                                combine(qc, c)

                        def combine(qc, c, h=h):
                            pv, pz = 2 + 2 * c, 3 + 2 * c
                            sc.op("dve", lambda e: e.reciprocal(out=cb[0][:], in_=PS[pz][:, :]), reads=[b_PS[pz]], writes=[b_cb[0]])
                            sc.op("dve", lambda e: e.tensor_tensor(out=cb[1 + c][:], in0=PS[pv][:, :], in1=cb[0][:], op=ALU.mult),
                                  reads=[b_PS[pv], b_cb[0]], writes=[b_cb[1 + c]])
                            if c == 0:
                                return
                            ci = ecs[2] % 2
                            ecs[2] += 1
                            o_, sq_ = cbo[ci], sqs[ci]
                            b_o, b_sq_ = b_cbo[ci], b_sqs[ci]
                            sc.op("dve", lambda e: e.scalar_tensor_tensor(out=o_[:], in0=cb[2][:], scalar=misc[:, 0:1], in1=cb[1][:],
                                                                          op0=ALU.mult, op1=ALU.add),
                                  reads=[b_cb[1], b_cb[2], b_misc], writes=[b_o])
                            sc.op("pool", lambda e: e.tensor_tensor(out=sq_[:], in0=o_[:], in1=o_[:], op=ALU.mult),
                                  reads=[b_o], writes=[b_sq_])

                            def tail():
                                mb = SB[(2 * ecs[0]) % 4]
                                sc.group("pe", [lambda e: e.matmul(PS[mb][:, :], ones[:], sq_[:], start=True, stop=True)],
                                         reads=[b_sq_, b_ident], writes=[b_PS[mb]])
                                sc.op("act", lambda e: e.activation(out=cb[4][:], in_=PS[mb][:, :], func=AF.Ln, scale=1.0 / 128, bias=SUBLN_EPS),
                                      reads=[b_PS[mb]], writes=[b_cb[4]])
                                sc.op("act", lambda e: e.activation(out=cb[4][:], in_=cb[4][:], func=AF.Exp, scale=-0.5),
                                      reads=[b_cb[4]], writes=[b_cb[4]])
                                sc.op("pool", lambda e: e.tensor_tensor(out=o_[:], in0=o_[:], in1=cb[4][:], op=ALU.mult),
                                      reads=[b_o, b_cb[4]], writes=[b_o])
                                sc.op("dve", lambda e: e.scalar_tensor_tensor(
                                    out=yaT[:, h, qc * 512:(qc + 1) * 512], in0=o_[:], scalar=misc[:, 1:2], in1=sza[:, qc * 512:(qc + 1) * 512],
                                    op0=ALU.mult, op1=ALU.mult), reads=[b_o, b_misc, b_sza], writes=[b_yaT[h][qc]])
                            deferred.append([4, tail])

                        def tick():
                            for d in list(deferred):
                                d[0] -= 1
                                if d[0] <= 0:
                                    deferred.remove(d)
                                    d[1]()

                        for (qc, c, kp) in pairs:
                            eis = emit_front(qc, c, kp)
                            pend.append((qc, c, kp, eis))
                            if len(pend) > 1:
                                emit_back(*pend.pop(0))
                            tick()
                        while pend:
                            emit_back(*pend.pop(0))
                        while deferred:
                            tick()
                    sc.barrier()

                with contextlib.ExitStack() as ph:
                    sbp = lambda name, shape, dty: _alloc(ph, name, shape, dty)
                    mT = sbp("mT", [P, KT, S], BF16)
                    Wpa = sbp("Wpa", [P, 4, D], BF16)
                    Wph = sbp("Wph", [P, 4, D], BF16)
                    Wout = sbp("Wout", [P, KT, D], BF16)
                    sg = [sbp(f"sg{i}", [P, 512], BF16) for i in range(4)]
                    tmpc = [sbp(f"tmpc{i}", [P, 512], F32) for i in range(2)]
                    pn = sbp("pn", [P, D], F32)
                    ot = [sbp(f"ot{i}", [P, D], F32) for i in range(2)]
                    xs = [sbp(f"xsc{i}", [P, D], F32) for i in range(2)]
                    junk = sbp("junkc", [P, D], BF16)
                    b_mT = [[Buf() for _ in range(4)] for _ in range(KT)]
                    b_Wp, b_pn = Buf(), Buf()
                    b_sg, b_tmpc, b_ot = [Buf() for _ in range(4)], [Buf(), Buf()], [Buf(), Buf()]
                    sc.dma("pool", Wpa[:], w_pa[l].rearrange("(kt p) n -> p kt n", p=P), s_a[0], writes=[b_Wp])
                    sc.dma("pool", Wph[:], w_ph[l].rearrange("(kt p) n -> p kt n", p=P), s_a[0], writes=[b_Wp])
                    sc.dma("pool", Wout[:, 0:4, :], w_out[l].rearrange("(kt p) n -> p kt n", p=P)[:, 0:4, :], s_a[0], writes=[b_Wp])
                    sc.dma("pool", Wout[:, 4:8, :], w_out[l].rearrange("(kt p) n -> p kt n", p=P)[:, 4:8, :], s_a[0], writes=[b_Wp])
                    sc.dma("sp", pn[:], rowb(l, 0, 1024), s_a[1], writes=[b_pn])
                    gct = 0
                    for ft in range(KT):
                        if ft % 2 == 0:
                            wga = load_w([(win_cols(l, 4096 + ft * 128, 256), 0, 256)])
                            wgh = load_w([(win_cols(l, 5120 + ft * 128, 256), 0, 256)])
                        goff = (ft % 2) * 128
                        for tc in range(4):
                            si = (gct % 2) * 2
                            ti = gct % 2
                            gct += 1
                            for gi, wi in ((0, wga), (1, wgh)):
                                pb = gi
                                sc.group("pe", [lambda e, kt=kt, wi=wi, tc=tc, pb=pb: e.matmul(
                                    PS[pb][:, :], Wsl[wi][:, kt, goff:goff + 128], hT[:, kt, tc * 512:(tc + 1) * 512],
                                    start=(kt == 0), stop=(kt == KT - 1)) for kt in range(KT)],
                                    reads=[b_W[wi]] + hTr(tc * 4, (tc + 1) * 4), writes=[b_PS[pb]])
                                sc.op("act", lambda e, si=si, gi=gi, pb=pb: e.activation(out=sg[si + gi][:], in_=PS[pb][:, :], func=AF.Sigmoid),
                                      reads=[b_PS[pb]], writes=[b_sg[si + gi]])
                            sc.group("pe", [lambda e, kt=kt, ft=ft, tc=tc: e.matmul(
                                PS[2][:, :], Wpa[:, kt, ft * 128:(ft + 1) * 128], yaT[:, kt, tc * 512:(tc + 1) * 512],
                                start=(kt == 0), stop=(kt == 3)) for kt in range(4)],
                                reads=[b_Wp] + [b_yaT[kt][tc] for kt in range(4)], writes=[b_PS[2]])
                            sc.group("pe", [lambda e, kt=kt, ft=ft, tc=tc: e.matmul(
                                PS[3][:, :], Wph[:, kt, ft * 128:(ft + 1) * 128], yhT[:, kt, tc * 512:(tc + 1) * 512],
                                start=(kt == 0), stop=(kt == 3)) for kt in range(4)],
                                reads=[b_Wp] + b_yhT, writes=[b_PS[3]])
                            sc.op("dve", lambda e, ti=ti, si=si: e.tensor_tensor(out=tmpc[ti][:], in0=PS[2][:, :], in1=sg[si][:], op=ALU.mult),
                                  reads=[b_PS[2], b_sg[si]], writes=[b_tmpc[ti]])
                            sc.op("dve", lambda e, ti=ti, si=si: e.tensor_tensor(out=sg[si][:], in0=PS[3][:, :], in1=sg[si + 1][:], op=ALU.mult),
                                  reads=[b_PS[3], b_sg[si + 1]], writes=[b_sg[si]])
                            sc.op("dve", lambda e, ti=ti, si=si, ft=ft, tc=tc: e.tensor_tensor(
                                out=mT[:, ft, tc * 512:(tc + 1) * 512], in0=tmpc[ti][:], in1=sg[si][:], op=ALU.add),
                                reads=[b_tmpc[ti], b_sg[si]], writes=[b_mT[ft][tc]])
                    sc.dma("sp", xs[0][:], xsrc[0:128, :], s_xs[0], writes=[b_xs[0]])
                    for tt in range(16):
                        k = tt % 2
                        c0 = 32 + 5 * k
                        bm = b_pm[k]
                        if tt + 1 < 16:
                            k1 = (tt + 1) % 2
                            sc.dma("sp", xs[k1][:], xsrc[(tt + 1) * 128:(tt + 2) * 128, :], s_xs[k1], writes=[b_xs[k1]])
                        sc.op("dve", lambda e: e.memset(misc[:, c0:c0 + 2], 0.0), writes=[bm])
                        for half in range(2):
                            pb = 4 + half
                            sc.group("pe", [lambda e, ft=ft, half=half, pb=pb: e.matmul(
                                PS[pb][:, :], mT[:, ft, tt * 128:(tt + 1) * 128], Wout[:, ft, half * 512:(half + 1) * 512],
                                start=(ft == 0), stop=(ft == KT - 1)) for ft in range(KT)],
                                reads=[b_Wp] + [b_mT[ft][tt // 4] for ft in range(KT)], writes=[b_PS[pb]])
                            sc.op("act", lambda e, half=half, pb=pb: e.activation(
                                out=ot[k][:, half * 512:(half + 1) * 512], in_=PS[pb][:, :], func=AF.Copy),
                                reads=[b_PS[pb]], writes=[b_ot[k]])
                            sc.op("act", lambda e, half=half, pb=pb: e.activation(
                                out=junk[:, 0:512], in_=PS[pb][:, :], func=AF.Square, accum_out=misc[:, c0 + half:c0 + half + 1]),
                                reads=[b_PS[pb]], writes=[b_junk, bm])
                        sc.op("dve", lambda e: e.tensor_tensor(out=misc[:, c0 + 2:c0 + 3], in0=misc[:, c0:c0 + 1], in1=misc[:, c0 + 1:c0 + 2], op=ALU.add),
                              reads=[bm], writes=[bm])
                        sc.op("act", lambda e: e.activation(out=misc[:, c0 + 3:c0 + 4], in_=misc[:, c0 + 2:c0 + 3], func=AF.Ln, scale=1.0 / D, bias=NORM_EPS),
                              reads=[bm], writes=[bm])
                        sc.op("act", lambda e: e.activation(out=misc[:, c0 + 4:c0 + 5], in_=misc[:, c0 + 3:c0 + 4], func=AF.Exp, scale=-0.5),
                              reads=[bm], writes=[bm])
                        sc.op("dve", lambda e: e.scalar_tensor_tensor(out=ot[k][:], in0=ot[k][:], scalar=misc[:, c0 + 4:c0 + 5], in1=pn[:],
                                                                      op0=ALU.mult, op1=ALU.mult),
                              reads=[b_ot[k], bm, b_pn], writes=[b_ot[k]])
                        sc.op("pool", lambda e: e.tensor_tensor(out=ot[k][:], in0=ot[k][:], in1=xs[k][:], op=ALU.add),
                              reads=[b_ot[k], b_xs[k]], writes=[b_ot[k]])
                        sc.dma("sp", y_d[b, tt * 128:(tt + 1) * 128, :], ot[k][:], s_st[k], reads=[b_ot[k]])
                    sc.barrier()
                bs_scope.close()
        sc.barrier()
        print("instructions emitted:", sc.ninst)
    return nc


_PROG = {}
_NL = DEPTH


def kernel(**inputs):
    inp = {k: np.ascontiguousarray(np.asarray(v)) for k, v in inputs.items()}
    c = _consts()
    colp, rowp = _pack_params(inp)
    nl = _NL
    if nl not in _PROG:
        _PROG[nl] = build_program(nl)
    nc = _PROG[nl]
    in_maps = []
    for r in range(NCORES):
        m = {
            "x": np.ascontiguousarray(inp["x"][r * NBC:(r + 1) * NBC]),
            "w_in": inp["w_in"], "w_pa": inp["w_pa"], "w_ph": inp["w_ph"], "w_out": inp["w_out"],
            "flt_w1": inp["flt_w1"], "flt_w2": inp["flt_w2"], "flt_w3": inp["flt_w3"], "flt_w_out": inp["flt_w_out"],
            "rel_bias": inp["rel_bias"], "colp": colp, "rowp": rowp,
            "fw": c["fw"], "iv": c["iv"], "zT": c["zT"], "negt": c["negt"], "onehot": c["onehot"], "ident": c["ident"],
        }
        in_maps.append(m)
    res = run_bass_kernel_spmd(nc, in_maps, core_ids=list(range(NCORES)))
    out = np.concatenate([np.asarray(r["y"]) for r in res.results], axis=0)
    return out.astype(np.float32)
```

```python
import math
import contextlib
import numpy as np
import ml_dtypes
import concourse.bass as bass
import concourse.mybir as mybir
from concourse.bass_utils import run_bass_kernel_spmd

F32 = mybir.dt.float32
BF16 = mybir.dt.bfloat16
I32 = mybir.dt.int32
AF = mybir.ActivationFunctionType
ALU = mybir.AluOpType
AX = mybir.AxisListType

P = 128
S = 2048
D = 1024
KT = 8
NBC = 2
NCORES = 8
DEPTH = 4
WCOLS = 6144
CW = 256
NH = 512 // CW
NFFT = 4096
R_B = 1280
TBW = 1152
NORM_EPS = 1e-6
SUBLN_EPS = 1e-5
TWO_PI = 2.0 * math.pi
NCOLP = 64
NROWP = 4352


class Sem:
    def __init__(self, h):
        self.h = h
        self.v = 0
        self.pending = []
        self.closed_at = 0


class Buf:
    __slots__ = ("w", "r")

    def __init__(self):
        self.w = {}
        self.r = {}


class Sched:
    def __init__(self, nc, es):
        self.nc = nc
        self.es = es
        self.E = {"pe": nc.tensor, "act": nc.scalar, "dve": nc.vector, "pool": nc.gpsimd, "sp": nc.sync}
        self.sem = {k: Sem(es.enter_context(nc.semaphore("e_" + k))) for k in self.E}
        self.seen = {k: {} for k in self.E}
        self.dsems = []
        self.ninst = 0

    def newsem(self, name):
        x = Sem(self.es.enter_context(self.nc.semaphore(name)))
        self.dsems.append(x)
        return x

    def _wait(self, eng, tok):
        sem, val, owner = tok
        if owner == eng and eng == "pe":
            return
        if self.seen[eng].get(id(sem), 0) >= val:
            return
        self.E[eng].wait_ge(sem.h, val)
        self.seen[eng][id(sem)] = val

    def _deps(self, eng, reads, writes):
        for b in reads:
            for t in b.w.values():
                self._wait(eng, t)
        for b in writes:
            for t in b.w.values():
                self._wait(eng, t)
            for t in b.r.values():
                self._wait(eng, t)

    def _done(self, tok, key, reads, writes):
        for b in reads:
            b.r[key] = tok
        for b in writes:
            b.w[key] = tok
            b.r = {}

    def op(self, eng, fn, reads=(), writes=()):
        self._deps(eng, reads, writes)
        inst = fn(self.E[eng])
        sem = self.sem[eng]
        sem.v += 1
        inst.then_inc(sem.h, 1)
        self.ninst += 1
        self._done((sem, sem.v, eng), id(sem), reads, writes)

    def group(self, eng, fns, reads=(), writes=()):
        self._deps(eng, reads, writes)
        for f in fns[:-1]:
            f(self.E[eng])
        inst = fns[-1](self.E[eng])
        sem = self.sem[eng]
        sem.v += 1
        inst.then_inc(sem.h, 1)
        self.ninst += len(fns)
        self._done((sem, sem.v, eng), id(sem), reads, writes)

    def dma(self, q, out, in_, sem, reads=(), writes=(), shared=False):
        self._deps(q, reads, writes)
        if shared:
            if sem.closed_at > 0:
                self._wait(q, (sem, sem.closed_at, None))
            sem.pending.extend(writes)
        self.E[q].dma_start(out=out, in_=in_).then_inc(sem.h, 16)
        sem.v += 16
        self.ninst += 1
        self._done((sem, sem.v, None), id(sem), reads, writes)

    def flush(self, sem):
        tok = (sem, sem.v, None)
        for b in sem.pending:
            b.w[id(sem)] = tok
        sem.pending = []
        sem.closed_at = sem.v

    def barrier(self):
        toks = [(sm, sm.v, None) for sm in list(self.sem.values()) + self.dsems if sm.v > 0]
        for e in self.E:
            for t in toks:
                self._wait(e, t)


def _t5_bucket_np(rel):
    nb = 16
    max_exact = 8
    ret = np.where(rel > 0, nb, 0)
    n = np.abs(rel)
    nf = np.maximum(n, 1).astype(np.float32)
    large = max_exact + (np.log(nf / np.float32(max_exact)) / np.float32(math.log(128 / max_exact))
                         * np.float32(nb - max_exact)).astype(np.int32)
    large = np.minimum(large, nb - 1)
    return ret + np.where(n < max_exact, n, large)


_CONST_CACHE = {}


def _consts():
    if _CONST_CACHE:
        return _CONST_CACHE
    s = np.arange(S, dtype=np.float64)
    f = np.arange(2048, dtype=np.float64) + 0.5
    theta = 2.0 * np.pi * np.outer(s, f) / NFFT
    fwm = np.concatenate([np.cos(theta), -np.sin(theta)], axis=1)
    fw = fwm.reshape(16, 128, 32, 128).transpose(2, 1, 0, 3)
    ivm = (2.0 / NFFT) * fwm.T
    iv = ivm.reshape(2, 16, 128, 16, 128).transpose(3, 0, 2, 1, 4).reshape(32, 128, 16, 128)
    _CONST_CACHE["fw"] = np.ascontiguousarray(fw).astype(ml_dtypes.bfloat16)
    _CONST_CACHE["iv"] = np.ascontiguousarray(iv).astype(ml_dtypes.bfloat16)
    L = S
    t = np.linspace(0.0, 1.0, L, dtype=np.float32)
    t_res = np.arange(L, dtype=np.float32)[:, None]
    fb = np.linspace(1e-4, 15, 16, dtype=np.float32)[None, :]
    ang = (np.float32(2.0 * math.pi) * t_res * fb / np.float32(L)).astype(np.float32)
    z = np.concatenate([t[:, None], np.cos(ang.astype(np.float64)), -np.sin(ang.astype(np.float64))], axis=-1)
    _CONST_CACHE["zT"] = np.ascontiguousarray(z.T).astype(np.float32)
    ng = np.zeros((128, 17), np.float32)
    ng[:, 0:16] = (-t).reshape(16, 128).T
    ng[:, 16] = 1.0 - 2.0 * (np.arange(128) % 2)
    _CONST_CACHE["negt"] = ng
    rel = 639 - np.arange(R_B)
    bk = _t5_bucket_np(rel)
    oh = np.zeros((32, R_B), np.float32)
    oh[bk, np.arange(R_B)] = 1.0
    _CONST_CACHE["onehot"] = oh
    _CONST_CACHE["ident"] = np.eye(128).astype(ml_dtypes.bfloat16)
    return _CONST_CACHE


def _pack_params(inp):
    L = DEPTH
    colp = np.zeros((L, 128, NCOLP), np.float32)
    rowp = np.zeros((L, NROWP), np.float32)
    for l in range(L):
        colp[l, :, 0:8] = inp["pre_norm"][l].reshape(8, 128).T
        cw = inp["conv_w"][l].reshape(3, 12, 128)
        colp[l, :, 8:44] = cw.transpose(2, 1, 0).reshape(128, 36)
        colp[l, :, 44:56] = inp["conv_b"][l].reshape(12, 128).T
        colp[l, :, 56] = inp["subln"][l]
        colp[l, :64, 57] = inp["flt_freq"][l]
        colp[l, :64, 58] = inp["flt_b1"][l]
        colp[l, :64, 59] = inp["flt_b2"][l]
        colp[l, :64, 60] = inp["flt_b3"][l]
        rowp[l, 0:1024] = inp["post_norm"][l]
        rowp[l, 1024:3072] = inp["flt_deltas"][l]
        rowp[l, 3072:4096] = inp["flt_bias"][l].reshape(-1)
        rowp[l, 4096:4160] = inp["lam_q1"][l]
        rowp[l, 4160:4224] = inp["lam_k1"][l]
        rowp[l, 4224:4288] = inp["lam_q2"][l]
        rowp[l, 4288:4352] = inp["lam_k2"][l]
    return colp, rowp


def build_program(nlayers=DEPTH):
    nc = bass.Bass("TRN2", target_bir_lowering=False)
    dt = nc.dram_tensor
    x_in = dt("x", [NBC, S, D], F32, kind="ExternalInput").ap()
    w_in = dt("w_in", [DEPTH, D, WCOLS], F32, kind="ExternalInput").ap()
    w_pa = dt("w_pa", [DEPTH, 512, D], F32, kind="ExternalInput").ap()
    w_ph = dt("w_ph", [DEPTH, 512, D], F32, kind="ExternalInput").ap()
    w_out = dt("w_out", [DEPTH, D, D], F32, kind="ExternalInput").ap()
    flt_w1 = dt("flt_w1", [DEPTH, 33, 64], F32, kind="ExternalInput").ap()
    flt_w2 = dt("flt_w2", [DEPTH, 64, 64], F32, kind="ExternalInput").ap()
    flt_w3 = dt("flt_w3", [DEPTH, 64, 64], F32, kind="ExternalInput").ap()
    flt_wo = dt("flt_w_out", [DEPTH, 64, 2048], F32, kind="ExternalInput").ap()
    rel_bias = dt("rel_bias", [32, 4], F32, kind="ExternalInput").ap()
    colp_d = dt("colp", [DEPTH, 128, NCOLP], F32, kind="ExternalInput").ap()
    rowp_h = dt("rowp", [DEPTH, NROWP], F32, kind="ExternalInput")
    rowp_d = rowp_h.ap()
    rowb = lambda l, a, n: bass.AP(tensor=rowp_h, offset=l * NROWP + a, ap=[[0, P], [1, n]])
    fw_d = dt("fw", [32, 128, 16, 128], BF16, kind="ExternalInput").ap()
    iv_d = dt("iv", [32, 128, 16, 128], BF16, kind="ExternalInput").ap()
    zT_d = dt("zT", [33, 2048], F32, kind="ExternalInput").ap()
    negt_d = dt("negt", [128, 17], F32, kind="ExternalInput").ap()
    oh_d = dt("onehot", [32, R_B], F32, kind="ExternalInput").ap()
    id_d = dt("ident", [128, 128], BF16, kind="ExternalInput").ap()
    y_d = dt("y", [NBC, S, D], F32, kind="ExternalOutput").ap()
    gs_h = dt("gs", [2, 16, 128, NH, 2 * CW], F32, kind="Internal")
    gs_d = gs_h.ap()
    bv_h = dt("bvec", [4, R_B], BF16, kind="Internal")

    with contextlib.ExitStack() as es:
        sc = Sched(nc, es)
        uid = [0]

        def _alloc(stack, name, shape, dty):
            uid[0] += 1
            return stack.enter_context(nc.sbuf_tensor(f"s{uid[0]}_{name}", shape, dty))

        sb = lambda name, shape, dty: _alloc(es, name, shape, dty)

        ident = sb("ident", [P, P], BF16)
        ones = sb("ones", [P, P], BF16)
        negt = sb("negt", [P, 17], F32)
        colp = sb("colp", [P, NCOLP], F32)
        misc = sb("misc", [P, 48], F32)
        TB = sb("TB", [P, 4, TBW], BF16)
        hT = sb("hT", [P, KT, S], BF16)
        yhT = sb("yhT", [P, 4, S], BF16)
        NW = 4
        Wsl = [sb(f"W{i}", [P, KT, 256], BF16) for i in range(NW)]
        b_hn2 = [Buf(), Buf()]
        b_pm = [Buf(), Buf()]
        PS = [es.enter_context(nc.psum_tensor(f"ps{i}", [P, 512], F32)) for i in range(8)]

        b_ident, b_negt, b_colp, b_misc, b_TB = Buf(), Buf(), Buf(), Buf(), Buf()
        b_hTa = [Buf() for _ in range(16)]
        b_hTb = [Buf() for _ in range(16)]
        hTr = lambda a, b: b_hTa[a:b] + b_hTb[a:b]
        b_yaT = [[Buf() for _ in range(4)] for _ in range(4)]
        b_yhT = [Buf() for _ in range(4)]
        b_W = [Buf() for _ in range(NW)]
        b_xs = [Buf(), Buf()]
        b_junk = Buf()
        b_PS = [Buf() for _ in range(8)]
        s_W = [sc.newsem(f"sW{i}") for i in range(NW)]
        s_xs = [sc.newsem(f"sxs{i}") for i in range(2)]
        s_c = sc.newsem("sconst")
        s_st = [sc.newsem(f"sst{i}") for i in range(2)]
        s_a = [sc.newsem(f"sa{i}") for i in range(8)]
        wctr = [0]

        def load_w(src_aps):
            i = wctr[0] % NW
            wctr[0] += 1
            for ap_, off, n in src_aps:
                sc.dma("pool", Wsl[i][:, :, off:off + n], ap_, s_W[i], writes=[b_W[i]])
            return i

        def win_cols(l, c0, n):
            return w_in[l].rearrange("(kt p) n -> p kt n", p=P)[:, :, c0:c0 + n]

        sc.dma("sp", ident[:], id_d[:, :], s_c, writes=[b_ident], shared=True)
        sc.dma("sp", negt[:], negt_d[:, :], s_c, writes=[b_negt], shared=True)
        sc.op("pool", lambda e: e.memset(ones[:], 1.0), writes=[b_ident])
        with contextlib.ExitStack() as ph:
            sbp = lambda name, shape, dty: _alloc(ph, name, shape, dty)
            rb = sbp("rb", [32, 4], F32)
            oh = sbp("oh", [32, R_B], F32)
            bvs = sbp("bvs", [4, R_B], BF16)
            b_rb, b_oh, b_bvs = Buf(), Buf(), Buf()
            sc.dma("sp", rb[:], rel_bias[:, :], s_c, writes=[b_rb], shared=True)
            sc.dma("sp", oh[:], oh_d[:, :], s_c, writes=[b_oh], shared=True)
            sc.flush(s_c)
            for j, (c0, n) in enumerate([(0, 512), (512, 512), (1024, 256)]):
                sc.group("pe", [lambda e, c0=c0, n=n, j=j: e.matmul(PS[j][0:4, 0:n], rb[:, :], oh[:, c0:c0 + n],
                                                                    start=True, stop=True)],
                         reads=[b_rb, b_oh], writes=[b_PS[j]])
                sc.op("dve", lambda e, c0=c0, n=n, j=j: e.tensor_copy(out=bvs[:, c0:c0 + n], in_=PS[j][0:4, 0:n]),
                      reads=[b_PS[j]], writes=[b_bvs])
            sc.dma("sp", bv_h.ap()[:, :], bvs[:], s_c, reads=[b_bvs], shared=True)
            sc.flush(s_c)
            sc.barrier()
            for p in range(P):
                src = bass.AP(tensor=bv_h, offset=127 - p, ap=[[0, 1], [R_B, 4], [1, TBW]])
                sc.dma("sp", TB[p:p + 1, :, :], src, s_c, writes=[b_TB], shared=True)
            sc.flush(s_c)
            sc.barrier()
            for h in range(4):
                sc.op("dve", lambda e, h=h: e.tensor_copy(out=misc[:, 22 + 2 * h:23 + 2 * h], in_=TB[:, h, 0:1]),
                      reads=[b_TB], writes=[b_misc])
                sc.op("dve", lambda e, h=h: e.tensor_copy(out=misc[:, 23 + 2 * h:24 + 2 * h], in_=TB[:, h, TBW - 1:TBW]),
                      reads=[b_TB], writes=[b_misc])

        for l in range(nlayers):
            lam_init = 0.8 - 0.6 * math.exp(-0.3 * l)
            sc.dma("sp", colp[:], colp_d[l], s_c, writes=[b_colp], shared=True)
            with contextlib.ExitStack() as ph:
                sbp = lambda name, shape, dty: _alloc(ph, name, shape, dty)
                lamv = sbp("lamv", [P, 256], F32)
                lamt = sbp("lamt", [P, 128], F32)
                b_lv, b_lt = Buf(), Buf()
                sc.dma("sp", lamv[:], rowb(l, 4096, 256), s_c, writes=[b_lv], shared=True)
                sc.flush(s_c)
                sc.op("dve", lambda e: e.tensor_tensor(out=lamt[:, 0:64], in0=lamv[:, 0:64], in1=lamv[:, 64:128], op=ALU.mult),
                      reads=[b_lv], writes=[b_lt])
                sc.op("dve", lambda e: e.tensor_tensor(out=lamt[:, 64:128], in0=lamv[:, 128:192], in1=lamv[:, 192:256], op=ALU.mult),
                      reads=[b_lv], writes=[b_lt])
                sc.op("dve", lambda e: e.reduce_sum(out=misc[:, 6:7], in_=lamt[:, 0:64], axis=AX.X), reads=[b_lt], writes=[b_misc])
                sc.op("dve", lambda e: e.reduce_sum(out=misc[:, 7:8], in_=lamt[:, 64:128], axis=AX.X), reads=[b_lt], writes=[b_misc])
                sc.op("act", lambda e: e.activation(out=misc[:, 8:10], in_=misc[:, 6:8], func=AF.Exp), reads=[b_misc], writes=[b_misc])
                sc.op("dve", lambda e: e.tensor_tensor(out=misc[:, 10:11], in0=misc[:, 9:10], in1=misc[:, 8:9], op=ALU.subtract),
                      reads=[b_misc], writes=[b_misc])
                sc.op("dve", lambda e: e.tensor_scalar(out=misc[:, 0:1], in0=misc[:, 10:11], scalar1=-lam_init, scalar2=None, op0=ALU.add),
                      reads=[b_misc], writes=[b_misc])
                sc.op("dve", lambda e: e.tensor_scalar(out=misc[:, 1:2], in0=colp[:, 56:57], scalar1=(1.0 - lam_init), scalar2=None, op0=ALU.mult),
                      reads=[b_colp, b_misc], writes=[b_misc])
                sc.op("dve", lambda e: e.tensor_scalar(out=misc[:, 2:3], in0=colp[:, 57:58], scalar1=1.0 / TWO_PI, scalar2=None, op0=ALU.mult),
                      reads=[b_colp, b_misc], writes=[b_misc])
                for j in range(3):
                    sc.op("dve", lambda e, j=j: e.tensor_scalar(out=misc[:, 3 + j:4 + j], in0=colp[:, 58 + j:59 + j],
                                                              scalar1=misc[:, 2:3], scalar2=16.0, op0=ALU.mult, op1=ALU.add),
                          reads=[b_colp, b_misc], writes=[b_misc])
                sc.barrier()

            with contextlib.ExitStack() as ph:
                sbp = lambda name, shape, dty: _alloc(ph, name, shape, dty)
                aA = sbp("aA", [64, S], F32)
                wo = sbp("wo", [64, 2048], F32)
                dabs = sbp("dabs", [P, 2048], F32)
                brow = sbp("brow", [1, 1024], F32)
                a3b = sbp("a3b", [64, S], BF16)
                wob = sbp("wob", [64, 2048], BF16)
                b_a3b, b_wob = Buf(), Buf()
                b_aA, b_wo, b_dabs, b_brow = Buf(), Buf(), Buf(), Buf()
                sc.dma("sp", wo[:], flt_wo[l], s_c, writes=[b_wo], shared=True)
                sc.dma("sp", dabs[:], rowb(l, 1024, 2048), s_c, writes=[b_dabs], shared=True)
                sc.dma("sp", brow[:], rowp_d[l:l + 1, 3072:4096], s_c, writes=[b_brow], shared=True)
                sc.flush(s_c)
                sc.op("act", lambda e: e.activation(out=dabs[:], in_=dabs[:], func=AF.Abs), reads=[b_dabs], writes=[b_dabs])
                with contextlib.ExitStack() as ph2:
                    sbq = lambda name, shape, dty: _alloc(ph2, name, shape, dty)
                    zT = sbq("zT", [33, S], F32)
                    aB = sbq("aB", [64, S], F32)
                    w1 = sbq("w1", [33, 64], F32)
                    w2 = sbq("w2", [64, 64], F32)
                    w3 = sbq("w3", [64, 64], F32)
                    tA = [sbq(f"tA{i}", [64, 512], F32) for i in range(2)]
                    tI = [sbq(f"tI{i}", [64, 512], I32) for i in range(2)]
                    tF = [sbq(f"tF{i}", [64, 512], F32) for i in range(2)]
                    b_zT, b_aB, b_w = Buf(), Buf(), Buf()
                    b_tA, b_tI, b_tF = [Buf(), Buf()], [Buf(), Buf()], [Buf(), Buf()]
                    sc.dma("sp", zT[:], zT_d[:, :], s_c, writes=[b_zT], shared=True)
                    sc.dma("sp", w1[:], flt_w1[l], s_c, writes=[b_w], shared=True)
                    sc.dma("sp", w2[:], flt_w2[l], s_c, writes=[b_w], shared=True)
                    sc.dma("sp", w3[:], flt_w3[l], s_c, writes=[b_w], shared=True)
                    sc.flush(s_c)
                    chain = [(w1, 33, zT, b_zT, aA, b_aA), (w2, 64, aA, b_aA, aB, b_aB), (w3, 64, aB, b_aB, aA, b_aA)]
                    it = 0
                    for j, (wj, kk, src, b_src, dst, b_dst) in enumerate(chain):
                        for ch in range(4):
                            sl = slice(ch * 512, (ch + 1) * 512)
                            k = it % 2
                            pb = it % 2
                            it += 1
                            sc.group("pe", [lambda e, wj=wj, kk=kk, src=src, sl=sl, pb=pb: e.matmul(
                                PS[pb][0:64, :], wj[0:kk, :], src[0:kk, sl], start=True, stop=True)],
                                reads=[b_w, b_src], writes=[b_PS[pb]])
                            sc.op("dve", lambda e, k=k, pb=pb, j=j: e.tensor_scalar(
                                out=tA[k][:], in0=PS[pb][0:64, :], scalar1=misc[0:64, 2:3], scalar2=misc[0:64, 3 + j:4 + j],
                                op0=ALU.mult, op1=ALU.add), reads=[b_PS[pb], b_misc], writes=[b_tA[k]])
                            sc.op("dve", lambda e, k=k: e.tensor_copy(out=tI[k][:], in_=tA[k][:]), reads=[b_tA[k]], writes=[b_tI[k]])
                            sc.op("dve", lambda e, k=k: e.tensor_copy(out=tF[k][:], in_=tI[k][:]), reads=[b_tI[k]], writes=[b_tF[k]])
                            sc.op("dve", lambda e, k=k: e.tensor_tensor(out=tA[k][:], in0=tA[k][:], in1=tF[k][:], op=ALU.subtract),
                                  reads=[b_tA[k], b_tF[k]], writes=[b_tA[k]])
                            sc.op("dve", lambda e, k=k: e.tensor_scalar(out=tA[k][:], in0=tA[k][:], scalar1=0.5, scalar2=-0.5,
                                                                      op0=ALU.min, op1=ALU.max),
                                  reads=[b_tA[k]], writes=[b_tA[k]])
                            sc.op("act", lambda e, k=k, dst=dst, sl=sl: e.activation(out=dst[:, sl], in_=tA[k][:], func=AF.Sin, scale=TWO_PI),
                                  reads=[b_tA[k]], writes=[b_dst])
                    sc.op("dve", lambda e: e.tensor_copy(out=a3b[:], in_=aA[:]), reads=[b_aA], writes=[b_a3b])
                    sc.op("pool", lambda e: e.tensor_copy(out=wob[:], in_=wo[:]), reads=[b_wo], writes=[b_wob])
                    sc.barrier()
                with contextlib.ExitStack() as ph2:
                    sbq = lambda name, shape, dty: _alloc(ph2, name, shape, dty)
                    FS = sbq("FS", [P, 16, NH, 2 * CW], BF16)
                    FD = sbq("FD", [P, 16, NH, 2 * CW], BF16)
                    dec = [sbq(f"dec{i}", [P, 512], F32) for i in range(2)]
                    hfb = [sbq(f"hfb{i}", [P, 512], F32) for i in range(2)]
                    FWs = [sbq(f"FWs{i}", [P, 16, 128], BF16) for i in range(3)]
                    Gst = [sbq(f"Gst{i}", [P, 2 * CW], F32) for i in range(2)]
                    b_FS, b_FD = [Buf() for _ in range(16)], [Buf() for _ in range(16)]
                    b_dec, b_hfb = [Buf(), Buf()], [Buf(), Buf()]
                    b_FWs, b_Gst = [Buf() for _ in range(3)], [Buf(), Buf()]
                    gc = 0
                    sgn = negt[:, 16:17]
                    for o in range(2):
                        for tt in range(16):
                            for di in range(2):
                                cs = slice(di * 1024 + o * 512, di * 1024 + (o + 1) * 512)
                                pb = di
                                sc.group("pe", [lambda e, tt=tt, cs=cs, pb=pb: e.matmul(
                                    PS[pb][:, :], a3b[:, tt * 128:(tt + 1) * 128], wob[:, cs], start=True, stop=True)],
                                    reads=[b_a3b, b_wob], writes=[b_PS[pb]])
                                sc.op("act", lambda e, di=di, cs=cs, tt=tt: e.activation(
                                    out=dec[di][:], in_=dabs[:, cs], func=AF.Exp, scale=negt[:, tt:tt + 1]),
                                    reads=[b_dabs, b_negt], writes=[b_dec[di]])
                                sc.op("dve", lambda e, di=di, pb=pb: e.tensor_tensor(out=hfb[di][:], in0=PS[pb][:, :], in1=dec[di][:], op=ALU.mult),
                                      reads=[b_PS[pb], b_dec[di]], writes=[b_hfb[di]])
                            if tt == 0:
                                sc.op("dve", lambda e: e.memset(hfb[1][0:1, :], 0.0), writes=[b_hfb[1]])
                                sc.op("dve", lambda e, o=o: e.tensor_tensor(out=hfb[0][0:1, :], in0=hfb[0][0:1, :],
                                                                          in1=brow[0:1, o * 512:(o + 1) * 512], op=ALU.add),
                                      reads=[b_brow], writes=[b_hfb[0]])
                            h0 = hfb[0][:, :].rearrange("p (h c) -> p h c", h=NH)
                            h1 = hfb[1][:, :].rearrange("p (h c) -> p h c", h=NH)
                            sc.op("pool", lambda e, tt=tt, h0=h0, h1=h1: e.tensor_tensor(out=FS[:, tt, :, 0:CW], in0=h0, in1=h1, op=ALU.add),
                                  reads=[b_hfb[0], b_hfb[1]], writes=[b_FS[tt]])
                            sc.op("dve", lambda e, tt=tt, h0=h0, h1=h1: e.tensor_tensor(out=FD[:, tt, :, 0:CW], in0=h0, in1=h1, op=ALU.subtract),
                                  reads=[b_hfb[0], b_hfb[1]], writes=[b_FD[tt]])
                            sc.op("act", lambda e, tt=tt: e.activation(out=FS[:, tt, :, CW:2 * CW], in_=FS[:, tt, :, 0:CW], func=AF.Identity, scale=sgn),
                                  reads=[b_negt], writes=[b_FS[tt]])
                            sc.op("act", lambda e, tt=tt: e.activation(out=FD[:, tt, :, CW:2 * CW], in_=FD[:, tt, :, 0:CW], func=AF.Identity, scale=sgn),
                                  reads=[b_negt], writes=[b_FD[tt]])
                        for hf in range(NH):

                            def fw_load(ct):
                                fi = ct % 3
                                sc.dma("sp", FWs[fi][:], fw_d[ct if ct < 8 else ct + 8], s_a[fi], writes=[b_FWs[fi]])
                            fw_load(0)
                            fw_load(1)
                            for ct in range(16):
                                fi = ct % 3
                                src, b_src = (FS, b_FS) if ct < 8 else (FD, b_FD)
                                pb = 2 + (ct % 2)
                                sc.group("pe", [lambda e, st=st, fi=fi, src=src, pb=pb: e.matmul(
                                    PS[pb][:, :], FWs[fi][:, st, :], src[:, st, hf, :], start=(st == 0), stop=(st == 15)) for st in range(16)],
                                    reads=[b_FWs[fi]] + b_src, writes=[b_PS[pb]])
                                gi = gc % 2
                                gc += 1
                                sc.op("act", lambda e, gi=gi, pb=pb: e.activation(out=Gst[gi][:], in_=PS[pb][:, :], func=AF.Copy),
                                      reads=[b_PS[pb]], writes=[b_Gst[gi]])
                                if ct + 2 < 16:
                                    fw_load(ct + 2)
                                sc.dma("sp", gs_d[o, ct, :, hf, :], Gst[gi][:], s_a[3 + gi], reads=[b_Gst[gi]])
                    sc.barrier()

            for b in range(NBC):
                xsrc = x_in[b] if l == 0 else y_d[b]
                pa_scope = contextlib.ExitStack()
                xs = [_alloc(pa_scope, f"xs{i}", [P, D], F32) for i in range(2)]
                hn2 = [_alloc(pa_scope, f"hn{i}", [P, D], BF16) for i in range(2)]
                junk = _alloc(pa_scope, "junk", [P, D], BF16)
                def pa_stage1(tt):
                    k = tt % 2
                    c0 = 12 + 3 * k
                    bm = b_pm[k]
                    sc.dma("sp", xs[k][:], xsrc[tt * 128:(tt + 1) * 128, :], s_xs[k], writes=[b_xs[k]])
                    sc.op("dve", lambda e: e.memset(misc[:, c0:c0 + 1], 0.0), writes=[bm])
                    sc.op("act", lambda e: e.activation(out=junk[:], in_=xs[k][:], func=AF.Square, accum_out=misc[:, c0:c0 + 1]),
                          reads=[b_xs[k]], writes=[b_junk, bm])
                    sc.op("act", lambda e: e.activation(out=misc[:, c0 + 1:c0 + 2], in_=misc[:, c0:c0 + 1], func=AF.Ln, scale=1.0 / D, bias=NORM_EPS),
                          reads=[bm], writes=[bm])
                    sc.op("act", lambda e: e.activation(out=misc[:, c0 + 2:c0 + 3], in_=misc[:, c0 + 1:c0 + 2], func=AF.Exp, scale=-0.5),
                          reads=[bm], writes=[bm])
                    sc.op("act", lambda e: e.activation(out=hn2[k][:], in_=xs[k][:], func=AF.Identity, scale=misc[:, c0 + 2:c0 + 3]),
                          reads=[b_xs[k], bm], writes=[b_hn2[k]])

                def pa_stage2(tt):
                    k = tt % 2
                    pb = 6 + (tt % 2)
                    pT = PS[pb][:, :].bitcast(BF16)
                    sc.group("pe", [lambda e, kt=kt: e.transpose(out=pT[:, kt * 128:(kt + 1) * 128],
                                                                 in_=hn2[k][:, kt * 128:(kt + 1) * 128], identity=ident[:])
                                    for kt in range(KT)], reads=[b_hn2[k], b_ident], writes=[b_PS[pb]])
                    for kt in range(KT):
                        sc.op("dve", lambda e, kt=kt: e.tensor_scalar(
                            out=hT[:, kt, tt * 128:(tt + 1) * 128], in0=pT[:, kt * 128:(kt + 1) * 128],
                            scalar1=colp[:, kt:kt + 1], scalar2=None, op0=ALU.mult),
                            reads=[b_PS[pb], b_colp], writes=[b_hTa[tt]])

                pa_stage1(0)
                for tt in range(16):
                    if tt + 1 < 16:
                        pa_stage1(tt + 1)
                    pa_stage2(tt)
                sc.barrier()
                pa_scope.close()

                for hf in range(NH):
                    with contextlib.ExitStack() as ph:
                        sbp = lambda name, shape, dty: _alloc(ph, name, shape, dty)
                        NCT = CW // 128
                        ust = [sbp(f"ust{i}", [P, S + 2], F32) for i in range(2)]
                        uc = [sbp(f"uc{i}", [P, S], BF16) for i in range(2)]
                        U = sbp("U", [P, 16, 2 * CW], BF16)
                        HX1 = sbp("HX1", [P, 16, CW], BF16)
                        G2 = sbp("G2", [P, 16, CW], BF16)
                        Y = sbp("Y", [P, 32 * CW], BF16)
                        DS = [sbp(f"DS{i}", [P, 16, 128], BF16) for i in range(4)]
                        Gsl = [sbp(f"Gsl{i}", [P, 2, 2 * CW], F32) for i in range(2)]
                        cm = [sbp(f"cm{i}", [P, 4, 2 * CW], F32) for i in range(2)]
                        yht = [sbp(f"yht{i}", [P, CW], BF16) for i in range(2)]
                        tA_ = [sbp(f"tA_{i}", [P, CW], F32) for i in range(2)]
                        yv = [sbp(f"yv{i}", [P, CW], F32) for i in range(2)]
                        b_tA_, b_yv = [Buf(), Buf()], [Buf(), Buf()]
                        sgn = negt[:, 16:17]
                        b_ust, b_uc = [Buf(), Buf()], [Buf(), Buf()]
                        b_U, b_HX1, b_G2 = [Buf() for _ in range(16)], [Buf() for _ in range(16)], [Buf() for _ in range(16)]
                        b_Y = [Buf() for _ in range(32)]
                        b_DS = [Buf() for _ in range(4)]
                        b_Gsl, b_cm, b_yht = [Buf(), Buf()], [Buf(), Buf()], [Buf(), Buf()]
                        for i in range(2):
                            sc.op("pool", lambda e, i=i: e.memset(ust[i][:, 0:1], 0.0), writes=[b_ust[i]])
                            sc.op("pool", lambda e, i=i: e.memset(ust[i][:, S + 1:S + 2], 0.0), writes=[b_ust[i]])
                        ui = 0
                        tpcs = [0]
                        pend_ct = []
                        for grp, (cbase, dstT, b_dst) in enumerate([(2048, U, b_U), (2560, HX1, b_HX1), (3072, G2, b_G2), (3584, G2, b_G2)]):
                            wi = load_w([(win_cols(l, cbase + hf * CW, CW), 0, CW)])
                            for j in range(NCT):
                                k = ui % 2
                                ui += 1
                                ctg = (cbase - 2048) // 128 + hf * NCT + j
                                for tc in range(4):
                                    pb = tc % 2
                                    sc.group("pe", [lambda e, kt=kt, wi=wi, j=j, tc=tc, pb=pb: e.matmul(
                                        PS[pb][:, :], Wsl[wi][:, kt, j * 128:(j + 1) * 128], hT[:, kt, tc * 512:(tc + 1) * 512],
                                        start=(kt == 0), stop=(kt == KT - 1)) for kt in range(KT)],
                                        reads=[b_W[wi]] + hTr(tc * 4, (tc + 1) * 4), writes=[b_PS[pb]])
                                    if grp < 3:
                                        sc.op("act", lambda e, k=k, tc=tc, pb=pb: e.activation(
                                            out=ust[k][:, 1 + tc * 512:1 + (tc + 1) * 512], in_=PS[pb][:, :], func=AF.Copy),
                                            reads=[b_PS[pb]], writes=[b_ust[k]])
                                    else:
                                        sc.op("act", lambda e, k=k, tc=tc, pb=pb: e.activation(
                                            out=uc[k][:, tc * 512:(tc + 1) * 512], in_=PS[pb][:, :], func=AF.Silu),
                                            reads=[b_PS[pb]], writes=[b_uc[k]])
                                if grp < 3:
                                    c0 = 8 + ctg * 3
                                    acc = Y[:, 0:2 * S].bitcast(F32)
                                    sc.op("act", lambda e, k=k, c0=c0, ctg=ctg, acc=acc: e.activation(
                                        out=acc[:, 0:S], in_=ust[k][:, 0:S], func=AF.Identity, scale=colp[:, c0:c0 + 1],
                                        bias=colp[:, 44 + ctg:45 + ctg]), reads=[b_ust[k], b_colp], writes=[b_cm[0]])
                                    sc.op("dve", lambda e, k=k, c0=c0, acc=acc: e.scalar_tensor_tensor(
                                        out=acc[:, 0:S], in0=ust[k][:, 1:S + 1], scalar=colp[:, c0 + 1:c0 + 2], in1=acc[:, 0:S],
                                        op0=ALU.mult, op1=ALU.add), reads=[b_ust[k], b_colp, b_cm[0]], writes=[b_cm[0]])
                                    sc.op("dve", lambda e, k=k, c0=c0, acc=acc: e.scalar_tensor_tensor(
                                        out=uc[k][:, :], in0=ust[k][:, 2:S + 2], scalar=colp[:, c0 + 2:c0 + 3], in1=acc[:, 0:S],
                                        op0=ALU.mult, op1=ALU.add), reads=[b_ust[k], b_colp, b_cm[0]], writes=[b_uc[k]])
                                def do_tr(k=k, j=j, grp=grp, dstT=dstT, b_dst=b_dst):
                                    for g8 in range(2):
                                        pb = 6 + (tpcs[0] % 2)
                                        tpcs[0] += 1
                                        pT = PS[pb][:, :].bitcast(BF16)
                                        sc.group("pe", [lambda e, k=k, i=i, g8=g8, pT=pT: e.transpose(
                                            out=pT[:, i * 128:(i + 1) * 128], in_=uc[k][:, (g8 * 8 + i) * 128:(g8 * 8 + i + 1) * 128],
                                            identity=ident[:]) for i in range(8)], reads=[b_uc[k], b_ident], writes=[b_PS[pb]])
                                        dsl = dstT[:, g8 * 8:(g8 + 1) * 8, j * 128:(j + 1) * 128]
                                        srcv = pT.rearrange("p (a b) -> p a b", a=8)
                                        if grp < 3:
                                            sc.op("act", lambda e, dsl=dsl, srcv=srcv: e.activation(out=dsl, in_=srcv, func=AF.Copy),
                                                  reads=[b_PS[pb]], writes=b_dst[g8 * 8:(g8 + 1) * 8])
                                            if grp == 0:
                                                dsm = dstT[:, g8 * 8:(g8 + 1) * 8, CW + j * 128:CW + (j + 1) * 128]
                                                sc.op("act", lambda e, dsl=dsl, dsm=dsm: e.activation(out=dsm, in_=dsl, func=AF.Identity, scale=sgn),
                                                      reads=[b_negt], writes=b_dst[g8 * 8:(g8 + 1) * 8])
                                        else:
                                            sc.op("dve", lambda e, dsl=dsl, srcv=srcv: e.tensor_tensor(out=dsl, in0=srcv, in1=dsl, op=ALU.mult),
                                                  reads=[b_PS[pb]], writes=b_dst[g8 * 8:(g8 + 1) * 8])
                                if pend_ct:
                                    pend_ct.pop(0)()
                                pend_ct.append(do_tr)
                        while pend_ct:
                            pend_ct.pop(0)()
                        dsc = 0
                        gsc = 0
                        cmc = 0
                        N2 = 2 * CW
                        pend_tr = []
                        for o in range(2):
                            for fp in range(8):
                                d0 = dsc % 4
                                d1 = (dsc + 1) % 4
                                dsc += 2
                                sc.dma("sp", DS[d0][:], fw_d[fp], s_a[d0], writes=[b_DS[d0]])
                                sc.dma("sp", DS[d1][:], fw_d[fp + 16], s_a[d1], writes=[b_DS[d1]])
                                gi = gsc % 2
                                gsc += 1
                                sc.dma("sp", Gsl[gi][:, 0, :], gs_d[o, fp, :, hf, :], s_a[4 + gi], writes=[b_Gsl[gi]])
                                sc.dma("sp", Gsl[gi][:, 1, :], gs_d[o, fp + 8, :, hf, :], s_a[4 + gi], writes=[b_Gsl[gi]])
                                for half, dd in ((0, d0), (1, d1)):
                                    pb = 2 + half + 2 * (fp % 2)
                                    sc.group("pe", [lambda e, st=st, dd=dd, pb=pb: e.matmul(
                                        PS[pb][:, 0:N2], DS[dd][:, st, :], U[:, st, :], start=(st == 0), stop=(st == 15)) for st in range(16)],
                                        reads=[b_DS[dd]] + b_U, writes=[b_PS[pb]])
                                pr = 2 + 2 * (fp % 2)
                                pi_ = pr + 1
                                ci = cmc % 2
                                cmc += 1
                                c = cm[ci]
                                sc.op("dve", lambda e, c=c, pr=pr, gi=gi: e.tensor_tensor(out=c[:, 0, :], in0=PS[pr][:, 0:N2], in1=Gsl[gi][:, 0, :], op=ALU.mult),
                                      reads=[b_PS[pr], b_Gsl[gi]], writes=[b_cm[ci]])
                                sc.op("dve", lambda e, c=c, pi_=pi_, gi=gi: e.tensor_tensor(out=c[:, 1, :], in0=PS[pi_][:, 0:N2], in1=Gsl[gi][:, 1, :], op=ALU.mult),
                                      reads=[b_PS[pi_], b_Gsl[gi]], writes=[b_cm[ci]])
                                sc.op("dve", lambda e, c=c, pr=pr, gi=gi: e.tensor_tensor(out=c[:, 2, :], in0=PS[pr][:, 0:N2], in1=Gsl[gi][:, 1, :], op=ALU.mult),
                                      reads=[b_PS[pr], b_Gsl[gi]], writes=[b_cm[ci]])
                                sc.op("dve", lambda e, c=c, pi_=pi_, gi=gi: e.tensor_tensor(out=c[:, 3, :], in0=PS[pi_][:, 0:N2], in1=Gsl[gi][:, 0, :], op=ALU.mult),
                                      reads=[b_PS[pi_], b_Gsl[gi]], writes=[b_cm[ci]])
                                sc.op("pool", lambda e, c=c, fp=fp: e.tensor_tensor(out=Y[:, fp * N2:(fp + 1) * N2], in0=c[:, 0, :], in1=c[:, 1, :], op=ALU.subtract),
                                      reads=[b_cm[ci]], writes=[b_Y[fp]])
                                sc.op("pool", lambda e, c=c, fp=fp: e.tensor_tensor(out=Y[:, (fp + 8) * N2:(fp + 9) * N2], in0=c[:, 2, :], in1=c[:, 3, :], op=ALU.add),
                                      reads=[b_cm[ci]], writes=[b_Y[fp + 8]])
                            for tt in range(16):
                                d0 = dsc % 4
                                d1 = (dsc + 1) % 4
                                dsc += 2
                                sc.dma("sp", DS[d0][:, 0:8, :], iv_d[tt * 2, :, 0:8, :], s_a[d0], writes=[b_DS[d0]])
                                sc.dma("sp", DS[d1][:, 0:8, :], iv_d[tt * 2 + 1, :, 0:8, :], s_a[d1], writes=[b_DS[d1]])
                                pb = 2 + (tt % 2)
                                fns = []
                                for hh, dd in ((0, d0), (1, d1)):
                                    for c_ in range(8):
                                        ct = hh * 8 + c_
                                        fns.append(lambda e, dd=dd, c_=c_, ct=ct, pb=pb: e.matmul(
                                            PS[pb][:, 0:N2], DS[dd][:, c_, :], Y[:, ct * N2:(ct + 1) * N2], start=(ct == 0), stop=(ct == 15)))
                                sc.group("pe", fns, reads=[b_DS[d0], b_DS[d1]] + b_Y[0:16], writes=[b_PS[pb]])
                                k = tt % 2
                                sc.op("act", lambda e, k=k, pb=pb: e.activation(out=tA_[k][:], in_=PS[pb][:, 0:CW], func=AF.Copy),
                                      reads=[b_PS[pb]], writes=[b_tA_[k]])
                                sc.op("dve", lambda e, k=k, pb=pb: e.scalar_tensor_tensor(
                                    out=yv[k][:], in0=PS[pb][:, CW:N2], scalar=sgn, in1=tA_[k][:], op0=ALU.mult, op1=ALU.add),
                                    reads=[b_PS[pb], b_tA_[k], b_negt], writes=[b_yv[k]])
                                if o == 0:
                                    sc.op("dve", lambda e, tt=tt, k=k: e.tensor_tensor(out=U[:, tt, 0:CW], in0=yv[k][:], in1=HX1[:, tt, :], op=ALU.mult),
                                          reads=[b_yv[k], b_HX1[tt]], writes=[b_U[tt]])
                                    sc.op("act", lambda e, tt=tt: e.activation(out=U[:, tt, CW:N2], in_=U[:, tt, 0:CW], func=AF.Identity, scale=sgn),
                                          reads=[b_negt], writes=[b_U[tt]])
                                else:
                                    sc.op("dve", lambda e, tt=tt, k=k: e.tensor_tensor(out=yht[k][:], in0=yv[k][:], in1=G2[:, tt, :], op=ALU.mult),
                                          reads=[b_yv[k], b_G2[tt]], writes=[b_yht[k]])
                                    def yh_tr(tt=tt, k=k):
                                        pq = 6 + (tt % 2)
                                        pT = PS[pq][:, :].bitcast(BF16)
                                        sc.group("pe", [lambda e, j=j: e.transpose(
                                            out=pT[:, j * 128:(j + 1) * 128], in_=yht[k][:, j * 128:(j + 1) * 128], identity=ident[:])
                                            for j in range(NCT)], reads=[b_yht[k], b_ident], writes=[b_PS[pq]])
                                        for j in range(NCT):
                                            cti = hf * NCT + j
                                            sc.op("act", lambda e, j=j, cti=cti: e.activation(
                                                out=yhT[:, cti, tt * 128:(tt + 1) * 128], in_=pT[:, j * 128:(j + 1) * 128], func=AF.Copy),
                                                reads=[b_PS[pq]], writes=[b_yhT[cti]])
                                    if pend_tr:
                                        pend_tr.pop(0)()
                                    pend_tr.append(yh_tr)
                            while pend_tr:
                                pend_tr.pop(0)()
                        sc.barrier()

                bs_scope = contextlib.ExitStack()
                yaT = _alloc(bs_scope, "yaT", [P, 4, S], BF16)
                with contextlib.ExitStack() as ph:
                    sbp = lambda name, shape, dty: _alloc(ph, name, shape, dty)
                    qT = sbp("qT", [P, S], BF16)
                    kTt = sbp("kTt", [P, S], BF16)
                    V = sbp("V", [P, 16, 128], BF16)
                    sza = sbp("sza", [P, S], BF16)
                    Es = [sbp(f"Es{i}", [P, 512], BF16) for i in range(4)]
                    tmpb = [sbp(f"tmpb{i}", [P, 512], F32) for i in range(2)]
                    cb = [sbp(f"cb{i}", [P, 512], F32) for i in range(5)]
                    sqs = [sbp(f"sq{i}", [P, 512], BF16) for i in range(2)]
                    cbo = [sbp(f"cbo{i}", [P, 512], F32) for i in range(2)]
                    b_sqs, b_cbo = [Buf(), Buf()], [Buf(), Buf()]
                    ecs = [0, 0, 0]
                    Ez = [sbp(f"Ez{i}", [P, 512], BF16) for i in range(2)]
                    b_Ez = [Buf(), Buf()]
                    b_qT, b_kT, b_V, b_sza = Buf(), Buf(), Buf(), Buf()
                    b_Es, b_tmpb = [Buf() for _ in range(4)], [Buf(), Buf()]
                    b_cb = [Buf() for _ in range(5)]
                    ec = 0
                    tc_ = 0
                    AW = [sbp(f"AW{i}", [P, KT, 256], BF16) for i in range(8)]
                    b_AW = [Buf() for _ in range(8)]
                    for hp in range(2):
                        for g in range(4):
                            i = hp * 4 + g
                            sc.dma("pool", AW[i][:], win_cols(l, g * 512 + hp * 256, 256), s_a[i], writes=[b_AW[i]])
                    for h in range(4):
                        hp, hoff = h // 2, (h % 2) * 128
                        for (wi, dst, b_dst, fn) in ((hp * 4 + 0, qT, b_qT, AF.Copy), (hp * 4 + 1, kTt, b_kT, AF.Copy), (hp * 4 + 3, sza, b_sza, AF.Silu)):
                            for tc in range(4):
                                pb = 6 + (tc % 2)
                                sc.group("pe", [lambda e, kt=kt, wi=wi, tc=tc, pb=pb: e.matmul(
                                    PS[pb][:, :], AW[wi][:, kt, hoff:hoff + 128], hT[:, kt, tc * 512:(tc + 1) * 512],
                                    start=(kt == 0), stop=(kt == KT - 1)) for kt in range(KT)],
                                    reads=[b_AW[wi]] + hTr(tc * 4, (tc + 1) * 4), writes=[b_PS[pb]])
                                sc.op("act", lambda e, dst=dst, tc=tc, pb=pb, fn=fn: e.activation(
                                    out=dst[:, tc * 512:(tc + 1) * 512], in_=PS[pb][:, :], func=fn),
                                    reads=[b_PS[pb]], writes=[b_dst])
                        for t4 in range(4):
                            pb = 6 + (t4 % 2)
                            fns = []
                            for i in range(4):
                                tt = t4 * 4 + i
                                for kt in range(KT):
                                    fns.append(lambda e, kt=kt, tt=tt, i=i, pb=pb: e.matmul(
                                        PS[pb][:, i * 128:(i + 1) * 128], hT[:, kt, tt * 128:(tt + 1) * 128], AW[hp * 4 + 2][:, kt, hoff:hoff + 128],
                                        start=(kt == 0), stop=(kt == KT - 1)))
                            sc.group("pe", fns, reads=[b_AW[hp * 4 + 2]] + hTr(t4 * 4, (t4 + 1) * 4), writes=[b_PS[pb]])
                            sc.op("dve", lambda e, t4=t4, pb=pb: e.tensor_copy(
                                out=V[:, t4 * 4:(t4 + 1) * 4, :], in_=PS[pb][:, :].rearrange("p (a b) -> p a b", a=4)),
                                reads=[b_PS[pb]], writes=[b_V])
                        SB = [0, 1, 6, 7]
                        pairs = [(qc, c, kp) for qc in range(4) for c in range(2) for kp in range(8)]
                        pend = []
                        deferred = []

                        def emit_front(qc, c, kp, h=h):
                            n = ecs[0]
                            ecs[0] += 1
                            banks = [SB[(2 * n) % 4], SB[(2 * n + 1) % 4]]
                            eis = [(2 * n) % 4, (2 * n + 1) % 4]
                            kts = [2 * kp, 2 * kp + 1]
                            sc.group("pe", [lambda e, kt=kt, pb=pb: e.matmul(
                                PS[pb][:, :], kTt[c * 64:(c + 1) * 64, kt * 128:(kt + 1) * 128],
                                qT[c * 64:(c + 1) * 64, qc * 512:(qc + 1) * 512], start=True, stop=True)
                                for kt, pb in zip(kts, banks)],
                                reads=[b_kT, b_qT], writes=[b_PS[banks[0]], b_PS[banks[1]]])
                            for kt, ps_, ei in zip(kts, banks, eis):
                                dlt = 128 * kt - 512 * qc
                                if dlt >= 640 or dlt <= -256:
                                    col = 22 + 2 * h + (0 if dlt >= 640 else 1)
                                    sc.op("act", lambda e: e.activation(
                                        out=Es[ei][:], in_=PS[ps_][:, :], func=AF.Exp, scale=0.125, bias=misc[:, col:col + 1]),
                                        reads=[b_PS[ps_], b_misc], writes=[b_Es[ei]])
                                else:
                                    ti = ecs[1] % 2
                                    ecs[1] += 1
                                    sc.op("dve", lambda e: e.scalar_tensor_tensor(
                                        out=tmpb[ti][:], in0=PS[ps_][:, :], scalar=0.125, in1=TB[:, h, 512 - dlt:1024 - dlt],
                                        op0=ALU.mult, op1=ALU.add), reads=[b_PS[ps_], b_TB], writes=[b_tmpb[ti]])
                                    sc.op("act", lambda e: e.activation(out=Es[ei][:], in_=tmpb[ti][:], func=AF.Exp),
                                          reads=[b_tmpb[ti]], writes=[b_Es[ei]])
                            return eis

                        def emit_back(qc, c, kp, eis, h=h):
                            pv, pz = 2 + 2 * c, 3 + 2 * c
                            fns = []
                            for kt, ei in zip([2 * kp, 2 * kp + 1], eis):
                                fns.append(lambda e, kt=kt, ei=ei: e.matmul(PS[pv][:, :], V[:, kt, :], Es[ei][:], start=(kt == 0), stop=(kt == 15)))
                                fns.append(lambda e, kt=kt, ei=ei: e.matmul(PS[pz][:, :], ones[:], Es[ei][:], start=(kt == 0), stop=(kt == 15)))
                            sc.group("pe", fns, reads=[b_V, b_Es[eis[0]], b_Es[eis[1]], b_ident], writes=[b_PS[pv], b_PS[pz]])
                            if kp == 7:
                                combine(qc, c)

                        def combine(qc, c, h=h):
                            pv, pz = 2 + 2 * c, 3 + 2 * c
                            sc.op("dve", lambda e: e.reciprocal(out=cb[0][:], in_=PS[pz][:, :]), reads=[b_PS[pz]], writes=[b_cb[0]])
                            sc.op("dve", lambda e: e.tensor_tensor(out=cb[1 + c][:], in0=PS[pv][:, :], in1=cb[0][:], op=ALU.mult),
                                  reads=[b_PS[pv], b_cb[0]], writes=[b_cb[1 + c]])
                            if c == 0:
                                return
                            ci = ecs[2] % 2
                            ecs[2] += 1
                            o_, sq_ = cbo[ci], sqs[ci]
                            b_o, b_sq_ = b_cbo[ci], b_sqs[ci]
                            sc.op("dve", lambda e: e.scalar_tensor_tensor(out=o_[:], in0=cb[2][:], scalar=misc[:, 0:1], in1=cb[1][:],
                                                                          op0=ALU.mult, op1=ALU.add),
                                  reads=[b_cb[1], b_cb[2], b_misc], writes=[b_o])
                            sc.op("pool", lambda e: e.tensor_tensor(out=sq_[:], in0=o_[:], in1=o_[:], op=ALU.mult),
                                  reads=[b_o], writes=[b_sq_])

                            def tail():
                                mb = SB[(2 * ecs[0]) % 4]
                                sc.group("pe", [lambda e: e.matmul(PS[mb][:, :], ones[:], sq_[:], start=True, stop=True)],
                                         reads=[b_sq_, b_ident], writes=[b_PS[mb]])
                                sc.op("act", lambda e: e.activation(out=cb[4][:], in_=PS[mb][:, :], func=AF.Ln, scale=1.0 / 128, bias=SUBLN_EPS),
                                      reads=[b_PS[mb]], writes=[b_cb[4]])
                                sc.op("act", lambda e: e.activation(out=cb[4][:], in_=cb[4][:], func=AF.Exp, scale=-0.5),
                                      reads=[b_cb[4]], writes=[b_cb[4]])
                                sc.op("pool", lambda e: e.tensor_tensor(out=o_[:], in0=o_[:], in1=cb[4][:], op=ALU.mult),
                                      reads=[b_o, b_cb[4]], writes=[b_o])
                                sc.op("dve", lambda e: e.scalar_tensor_tensor(
                                    out=yaT[:, h, qc * 512:(qc + 1) * 512], in0=o_[:], scalar=misc[:, 1:2], in1=sza[:, qc * 512:(qc + 1) * 512],
                                    op0=ALU.mult, op1=ALU.mult), reads=[b_o, b_misc, b_sza], writes=[b_yaT[h][qc]])
                            deferred.append([4, tail])

                        def tick():
                            for d in list(deferred):
                                d[0] -= 1
                                if d[0] <= 0:
                                    deferred.remove(d)
                                    d[1]()

                        for (qc, c, kp) in pairs:
                            eis = emit_front(qc, c, kp)
                            pend.append((qc, c, kp, eis))
                            if len(pend) > 1:
                                emit_back(*pend.pop(0))
                            tick()
                        while pend:
                            emit_back(*pend.pop(0))
                        while deferred:
                            tick()
                    sc.barrier()

                with contextlib.ExitStack() as ph:
                    sbp = lambda name, shape, dty: _alloc(ph, name, shape, dty)
                    mT = sbp("mT", [P, KT, S], BF16)
                    Wpa = sbp("Wpa", [P, 4, D], BF16)
                    Wph = sbp("Wph", [P, 4, D], BF16)
                    Wout = sbp("Wout", [P, KT, D], BF16)
                    sg = [sbp(f"sg{i}", [P, 512], BF16) for i in range(4)]
                    tmpc = [sbp(f"tmpc{i}", [P, 512], F32) for i in range(2)]
                    pn = sbp("pn", [P, D], F32)
                    ot = [sbp(f"ot{i}", [P, D], F32) for i in range(2)]
                    xs = [sbp(f"xsc{i}", [P, D], F32) for i in range(2)]
                    junk = sbp("junkc", [P, D], BF16)
                    b_mT = [[Buf() for _ in range(4)] for _ in range(KT)]
                    b_Wp, b_pn = Buf(), Buf()
                    b_sg, b_tmpc, b_ot = [Buf() for _ in range(4)], [Buf(), Buf()], [Buf(), Buf()]
                    sc.dma("pool", Wpa[:], w_pa[l].rearrange("(kt p) n -> p kt n", p=P), s_a[0], writes=[b_Wp])
                    sc.dma("pool", Wph[:], w_ph[l].rearrange("(kt p) n -> p kt n", p=P), s_a[0], writes=[b_Wp])
                    sc.dma("pool", Wout[:, 0:4, :], w_out[l].rearrange("(kt p) n -> p kt n", p=P)[:, 0:4, :], s_a[0], writes=[b_Wp])
                    sc.dma("pool", Wout[:, 4:8, :], w_out[l].rearrange("(kt p) n -> p kt n", p=P)[:, 4:8, :], s_a[0], writes=[b_Wp])
                    sc.dma("sp", pn[:], rowb(l, 0, 1024), s_a[1], writes=[b_pn])
                    gct = 0
                    for ft in range(KT):
                        if ft % 2 == 0:
                            wga = load_w([(win_cols(l, 4096 + ft * 128, 256), 0, 256)])
                            wgh = load_w([(win_cols(l, 5120 + ft * 128, 256), 0, 256)])
                        goff = (ft % 2) * 128
                        for tc in range(4):
                            si = (gct % 2) * 2
                            ti = gct % 2
                            gct += 1
                            for gi, wi in ((0, wga), (1, wgh)):
                                pb = gi
                                sc.group("pe", [lambda e, kt=kt, wi=wi, tc=tc, pb=pb: e.matmul(
                                    PS[pb][:, :], Wsl[wi][:, kt, goff:goff + 128], hT[:, kt, tc * 512:(tc + 1) * 512],
                                    start=(kt == 0), stop=(kt == KT - 1)) for kt in range(KT)],
                                    reads=[b_W[wi]] + hTr(tc * 4, (tc + 1) * 4), writes=[b_PS[pb]])
                                sc.op("act", lambda e, si=si, gi=gi, pb=pb: e.activation(out=sg[si + gi][:], in_=PS[pb][:, :], func=AF.Sigmoid),
                                      reads=[b_PS[pb]], writes=[b_sg[si + gi]])
                            sc.group("pe", [lambda e, kt=kt, ft=ft, tc=tc: e.matmul(
                                PS[2][:, :], Wpa[:, kt, ft * 128:(ft + 1) * 128], yaT[:, kt, tc * 512:(tc + 1) * 512],
                                start=(kt == 0), stop=(kt == 3)) for kt in range(4)],
                                reads=[b_Wp] + [b_yaT[kt][tc] for kt in range(4)], writes=[b_PS[2]])
                            sc.group("pe", [lambda e, kt=kt, ft=ft, tc=tc: e.matmul(
                                PS[3][:, :], Wph[:, kt, ft * 128:(ft + 1) * 128], yhT[:, kt, tc * 512:(tc + 1) * 512],
                                start=(kt == 0), stop=(kt == 3)) for kt in range(4)],
                                reads=[b_Wp] + b_yhT, writes=[b_PS[3]])
                            sc.op("dve", lambda e, ti=ti, si=si: e.tensor_tensor(out=tmpc[ti][:], in0=PS[2][:, :], in1=sg[si][:], op=ALU.mult),
                                  reads=[b_PS[2], b_sg[si]], writes=[b_tmpc[ti]])
                            sc.op("dve", lambda e, ti=ti, si=si: e.tensor_tensor(out=sg[si][:], in0=PS[3][:, :], in1=sg[si + 1][:], op=ALU.mult),
                                  reads=[b_PS[3], b_sg[si + 1]], writes=[b_sg[si]])
                            sc.op("dve", lambda e, ti=ti, si=si, ft=ft, tc=tc: e.tensor_tensor(
                                out=mT[:, ft, tc * 512:(tc + 1) * 512], in0=tmpc[ti][:], in1=sg[si][:], op=ALU.add),
                                reads=[b_tmpc[ti], b_sg[si]], writes=[b_mT[ft][tc]])
                    sc.dma("sp", xs[0][:], xsrc[0:128, :], s_xs[0], writes=[b_xs[0]])
                    for tt in range(16):
                        k = tt % 2
                        c0 = 32 + 5 * k
                        bm = b_pm[k]
                        if tt + 1 < 16:
                            k1 = (tt + 1) % 2
                            sc.dma("sp", xs[k1][:], xsrc[(tt + 1) * 128:(tt + 2) * 128, :], s_xs[k1], writes=[b_xs[k1]])
                        sc.op("dve", lambda e: e.memset(misc[:, c0:c0 + 2], 0.0), writes=[bm])
                        for half in range(2):
                            pb = 4 + half
                            sc.group("pe", [lambda e, ft=ft, half=half, pb=pb: e.matmul(
                                PS[pb][:, :], mT[:, ft, tt * 128:(tt + 1) * 128], Wout[:, ft, half * 512:(half + 1) * 512],
                                start=(ft == 0), stop=(ft == KT - 1)) for ft in range(KT)],
                                reads=[b_Wp] + [b_mT[ft][tt // 4] for ft in range(KT)], writes=[b_PS[pb]])
                            sc.op("act", lambda e, half=half, pb=pb: e.activation(
                                out=ot[k][:, half * 512:(half + 1) * 512], in_=PS[pb][:, :], func=AF.Copy),
                                reads=[b_PS[pb]], writes=[b_ot[k]])
                            sc.op("act", lambda e, half=half, pb=pb: e.activation(
                                out=junk[:, 0:512], in_=PS[pb][:, :], func=AF.Square, accum_out=misc[:, c0 + half:c0 + half + 1]),
                                reads=[b_PS[pb]], writes=[b_junk, bm])
                        sc.op("dve", lambda e: e.tensor_tensor(out=misc[:, c0 + 2:c0 + 3], in0=misc[:, c0:c0 + 1], in1=misc[:, c0 + 1:c0 + 2], op=ALU.add),
                              reads=[bm], writes=[bm])
                        sc.op("act", lambda e: e.activation(out=misc[:, c0 + 3:c0 + 4], in_=misc[:, c0 + 2:c0 + 3], func=AF.Ln, scale=1.0 / D, bias=NORM_EPS),
                              reads=[bm], writes=[bm])
                        sc.op("act", lambda e: e.activation(out=misc[:, c0 + 4:c0 + 5], in_=misc[:, c0 + 3:c0 + 4], func=AF.Exp, scale=-0.5),
                              reads=[bm], writes=[bm])
                        sc.op("dve", lambda e: e.scalar_tensor_tensor(out=ot[k][:], in0=ot[k][:], scalar=misc[:, c0 + 4:c0 + 5], in1=pn[:],
                                                                      op0=ALU.mult, op1=ALU.mult),
                              reads=[b_ot[k], bm, b_pn], writes=[b_ot[k]])
                        sc.op("pool", lambda e: e.tensor_tensor(out=ot[k][:], in0=ot[k][:], in1=xs[k][:], op=ALU.add),
                              reads=[b_ot[k], b_xs[k]], writes=[b_ot[k]])
                        sc.dma("sp", y_d[b, tt * 128:(tt + 1) * 128, :], ot[k][:], s_st[k], reads=[b_ot[k]])
                    sc.barrier()
                bs_scope.close()
        sc.barrier()
        print("instructions emitted:", sc.ninst)
    return nc


_PROG = {}
_NL = DEPTH


def kernel(**inputs):
    inp = {k: np.ascontiguousarray(np.asarray(v)) for k, v in inputs.items()}
    c = _consts()
    colp, rowp = _pack_params(inp)
    nl = _NL
    if nl not in _PROG:
        _PROG[nl] = build_program(nl)
    nc = _PROG[nl]
    in_maps = []
    for r in range(NCORES):
        m = {
            "x": np.ascontiguousarray(inp["x"][r * NBC:(r + 1) * NBC]),
            "w_in": inp["w_in"], "w_pa": inp["w_pa"], "w_ph": inp["w_ph"], "w_out": inp["w_out"],
            "flt_w1": inp["flt_w1"], "flt_w2": inp["flt_w2"], "flt_w3": inp["flt_w3"], "flt_w_out": inp["flt_w_out"],
            "rel_bias": inp["rel_bias"], "colp": colp, "rowp": rowp,
            "fw": c["fw"], "iv": c["iv"], "zT": c["zT"], "negt": c["negt"], "onehot": c["onehot"], "ident": c["ident"],
        }
        in_maps.append(m)
    res = run_bass_kernel_spmd(nc, in_maps, core_ids=list(range(NCORES)))
    out = np.concatenate([np.asarray(r["y"]) for r in res.results], axis=0)
    return out.astype(np.float32)
```
